# Optimizing a Trainium2 kernel written in Bass

```python
import jax, jax.numpy as jnp
from jax import lax
import numpy as np

D_MODEL = 1024
BATCH = 8
SEQ = 4096
DEPTH = 4

CHUNK = 64
N_MIXERS = 2
EPS = 1e-6
D_FF = 256 * ((8 * D_MODEL // 3 + 255) // 256)
M_HEADS = 8
M_QK = D_MODEL // 2
M_V = D_MODEL
M_DK = M_QK // M_HEADS
M_DV = M_V // M_HEADS
M_IN = 2 * M_QK + 2 * M_V + 2 * M_HEADS
R_WIDTH = 128 * ((4 * D_MODEL // 3) // 128)
R_BLOCKS = 8
R_BW = R_WIDTH // R_BLOCKS
CONV_W = 4
RG_C = 8.0

N_M_LAYERS = (DEPTH + 1) // 2
N_R_LAYERS = DEPTH // 2

kernel_name = "hybrid_mlstm_rglru_macaron"


def rmsnorm(x, g):
    xf = x.astype(jnp.float32)
    y = xf * lax.rsqrt(jnp.mean(xf * xf, axis=-1, keepdims=True) + EPS)
    return (y * g.astype(jnp.float32)).astype(x.dtype)


def swiglu(x, w_in, w_out):
    gate, up = jnp.split(x @ w_in, 2, axis=-1)
    return (jax.nn.silu(gate) * up) @ w_out


def mlstm_chunkwise(q, k, v, ig, lf):
    B, S, H, DK = q.shape
    DV = v.shape[-1]
    NC = S // CHUNK

    def to_chunks(t):
        t = t.reshape((B, NC, CHUNK, H) + t.shape[3:])
        return jnp.moveaxis(t, (1, 3), (0, 2))

    causal = jnp.tril(jnp.ones((CHUNK, CHUNK), dtype=bool))

    def step(carry, inp):
        C, n, m = carry
        qc, kc, vc, ic, fc = inp
        b = jnp.cumsum(fc, axis=-1)
        dmat = b[..., :, None] - b[..., None, :] + ic[..., None, :]
        dmat = jnp.where(causal, dmat, -jnp.inf)
        m_inter = b + m[..., None]
        m_t = jnp.maximum(m_inter, jnp.max(dmat, axis=-1))
        s = jnp.einsum('bhtk,bhsk->bhts', qc, kc) * jnp.exp(dmat - m_t[..., None])
        w_inter = jnp.exp(m_inter - m_t)
        num = (jnp.einsum('bhts,bhsv->bhtv', s, vc)
               + w_inter[..., None] * jnp.einsum('bhtk,bhkv->bhtv', qc, C))
        den = jnp.sum(s, axis=-1) + w_inter * jnp.einsum('bhtk,bhk->bht', qc, n)
        h = num / jnp.maximum(jnp.abs(den), jnp.exp(-m_t))[..., None]
        b_last = b[..., -1]
        g = b_last[..., None] - b + ic
        m_new = jnp.maximum(b_last + m, jnp.max(g, axis=-1))
        decay = jnp.exp(b_last + m - m_new)
        wk = jnp.exp(g - m_new[..., None])
        C_new = decay[..., None, None] * C + jnp.einsum('bhs,bhsk,bhsv->bhkv', wk, kc, vc)
        n_new = decay[..., None] * n + jnp.einsum('bhs,bhsk->bhk', wk, kc)
        return (C_new, n_new, m_new), h

    init = (jnp.zeros((B, H, DK, DV), jnp.float32),
            jnp.zeros((B, H, DK), jnp.float32),
            jnp.zeros((B, H), jnp.float32))
    xs = (to_chunks(q), to_chunks(k), to_chunks(v), to_chunks(ig), to_chunks(lf))
    _, h = lax.scan(step, init, xs)
    h = jnp.moveaxis(h, (0, 2), (1, 3))
    return h.reshape(B, S, H, DV)


def mlstm_mixer(x, w_in, b_i, b_f, head_norm, w_out):
    B, S, _ = x.shape
    z = x @ w_in
    q, k, v, o, ig, fg = jnp.split(
        z, [M_QK, 2 * M_QK, 2 * M_QK + M_V, 2 * M_QK + 2 * M_V, 2 * M_QK + 2 * M_V + M_HEADS],
        axis=-1)
    q = q.astype(jnp.float32).reshape(B, S, M_HEADS, M_DK) * (M_DK ** -0.5)
    k = k.astype(jnp.float32).reshape(B, S, M_HEADS, M_DK)
    v = v.astype(jnp.float32).reshape(B, S, M_HEADS, M_DV)
    ig = ig.astype(jnp.float32) + b_i.astype(jnp.float32)
    lf = jax.nn.log_sigmoid(fg.astype(jnp.float32) + b_f.astype(jnp.float32))
    h = mlstm_chunkwise(q, k, v, ig, lf)
    h = h * lax.rsqrt(jnp.mean(h * h, axis=-1, keepdims=True) + EPS)
    h = h.reshape(B, S, M_V) * head_norm.astype(jnp.float32)
    y = jax.nn.sigmoid(o) * h.astype(x.dtype)
    return y @ w_out


def _lin_combine(l, r):
    a_l, b_l = l
    a_r, b_r = r
    return a_l * a_r, a_r * b_l + b_r


def rglru_mixer(x, w_in, conv_w, conv_b, w_a, b_a, w_i, b_i, lam, w_out):
    B, S, _ = x.shape
    u, gate = jnp.split(x @ w_in, 2, axis=-1)
    u = lax.conv_general_dilated(u, conv_w, window_strides=(1,), padding=[(CONV_W - 1, 0)],
                                 dimension_numbers=('NWC', 'WIO', 'NWC'),
                                 feature_group_count=R_WIDTH) + conv_b
    ub = u.reshape(B, S, R_BLOCKS, R_BW)
    r = jax.nn.sigmoid(jnp.einsum('bsnc,ncd->bsnd', ub, w_a).reshape(B, S, R_WIDTH) + b_a)
    i = jax.nn.sigmoid(jnp.einsum('bsnc,ncd->bsnd', ub, w_i).reshape(B, S, R_WIDTH) + b_i)
    log_a = -RG_C * r.astype(jnp.float32) * jax.nn.softplus(-lam.astype(jnp.float32))
    a = jnp.exp(log_a)
    bterm = jnp.sqrt(-jnp.expm1(2.0 * log_a)) * (i * u).astype(jnp.float32)
    _, h = lax.associative_scan(_lin_combine, (a, bterm), axis=1)
    y = h.astype(x.dtype) * jax.nn.gelu(gate)
    return y @ w_out


def setup_inputs(seed: int = 0) -> dict:
    key = jax.random.key(seed)
    ks = jax.random.split(key, 32)
    f32 = jnp.float32

    def nrm(k, shape, scale):
        return jax.random.normal(k, shape, f32) * scale

    def gain(k, shape):
        return 1.0 + 0.1 * jax.random.normal(k, shape, f32)

    p_a = jax.random.uniform(ks[20], (N_R_LAYERS, R_WIDTH), f32, 0.9, 0.999) ** (1.0 / RG_C)
    return {
        "x": nrm(ks[0], (BATCH, SEQ, D_MODEL), 1.0),
        "ff1_norm": gain(ks[1], (DEPTH, D_MODEL)),
        "ff1_w_in": nrm(ks[2], (DEPTH, D_MODEL, 2 * D_FF), D_MODEL ** -0.5),
        "ff1_w_out": nrm(ks[3], (DEPTH, D_FF, D_MODEL), D_FF ** -0.5),
        "mix_norm": gain(ks[4], (DEPTH, D_MODEL)),
        "ff2_norm": gain(ks[5], (DEPTH, D_MODEL)),
        "ff2_w_in": nrm(ks[6], (DEPTH, D_MODEL, 2 * D_FF), D_MODEL ** -0.5),
        "ff2_w_out": nrm(ks[7], (DEPTH, D_FF, D_MODEL), D_FF ** -0.5),
        "m_w_in": nrm(ks[8], (N_M_LAYERS, D_MODEL, M_IN), D_MODEL ** -0.5),
        "m_b_i": nrm(ks[9], (N_M_LAYERS, M_HEADS), 0.1),
        "m_b_f": jnp.linspace(3.0, 6.0, M_HEADS, dtype=f32)[None, :] + nrm(ks[10], (N_M_LAYERS, M_HEADS), 0.1),
        "m_head_norm": gain(ks[11], (N_M_LAYERS, M_V)),
        "m_w_out": nrm(ks[12], (N_M_LAYERS, M_V, D_MODEL), M_V ** -0.5),
        "r_w_in": nrm(ks[13], (N_R_LAYERS, D_MODEL, 2 * R_WIDTH), D_MODEL ** -0.5),
        "r_conv_w": nrm(ks[14], (N_R_LAYERS, CONV_W, 1, R_WIDTH), CONV_W ** -0.5),
        "r_conv_b": nrm(ks[15], (N_R_LAYERS, R_WIDTH), 0.01),
        "r_w_a": nrm(ks[16], (N_R_LAYERS, R_BLOCKS, R_BW, R_BW), R_BW ** -0.5),
        "r_b_a": nrm(ks[17], (N_R_LAYERS, R_WIDTH), 0.01),
        "r_w_i": nrm(ks[18], (N_R_LAYERS, R_BLOCKS, R_BW, R_BW), R_BW ** -0.5),
        "r_b_i": nrm(ks[19], (N_R_LAYERS, R_WIDTH), 0.01),
        "r_lam": jnp.log(p_a) - jnp.log1p(-p_a),
        "r_w_out": nrm(ks[21], (N_R_LAYERS, R_WIDTH, D_MODEL), R_WIDTH ** -0.5),
        "final_norm": gain(ks[22], (D_MODEL,)),
    }


def reference(x, ff1_norm, ff1_w_in, ff1_w_out, mix_norm, ff2_norm, ff2_w_in, ff2_w_out,
              m_w_in, m_b_i, m_b_f, m_head_norm, m_w_out,
              r_w_in, r_conv_w, r_conv_b, r_w_a, r_b_a, r_w_i, r_b_i, r_lam, r_w_out,
              final_norm):
    for layer in range(DEPTH):
        x = x + 0.5 * swiglu(rmsnorm(x, ff1_norm[layer]), ff1_w_in[layer], ff1_w_out[layer])
        h = rmsnorm(x, mix_norm[layer])
        j = layer // N_MIXERS
        if layer % N_MIXERS == 0:
            h = mlstm_mixer(h, m_w_in[j], m_b_i[j], m_b_f[j], m_head_norm[j], m_w_out[j])
        else:
            h = rglru_mixer(h, r_w_in[j], r_conv_w[j], r_conv_b[j], r_w_a[j], r_b_a[j],
                            r_w_i[j], r_b_i[j], r_lam[j], r_w_out[j])
        x = x + h
        x = x + 0.5 * swiglu(rmsnorm(x, ff2_norm[layer]), ff2_w_in[layer], ff2_w_out[layer])
    return rmsnorm(x, final_norm)
```

```python
import numpy as np
from contextlib import ExitStack
import concourse.bass as bass
import concourse.mybir as mybir
from concourse.bass_utils import run_bass_kernel_spmd

F32 = mybir.dt.float32
BF16 = mybir.dt.bfloat16
AF = mybir.ActivationFunctionType
ALU = mybir.AluOpType
AX = mybir.AxisListType

D = 1024
DFF = 2816
SEQ = 4096
T = 512
NCH = 8
NJ = 22
HEADS = 8
RW = 1280
RC = 10
EPS = 1e-6
NB = 4
SLABW = 5632
VW = 132
NDMASEM = 24
SAME_ENG_SYNC = True

PRM_OFF = {}
def _prm_layout():
    off = 0
    for name, n in (("ff1_norm", 32), ("mix_norm", 32), ("ff2_norm", 32), ("final_norm", 8),
                    ("m_head_norm", 16), ("m_b_i", 2), ("m_b_f", 2), ("r_conv_w", 80),
                    ("r_conv_b", 20), ("r_b_a", 20), ("r_b_i", 20), ("r_lam", 20)):
        PRM_OFF[name] = off
        off += n
    return off
NPRM = _prm_layout()

C_ID, C_ONES, C_MASK, C_RMASK, C_MISC = 0, 128, 256, 320, 832
NCST = 848


def pack_consts():
    c = np.zeros((128, NCST), np.float32)
    c[:, C_ID:C_ID + 128] = np.eye(128, dtype=np.float32)
    c[:, C_ONES:C_ONES + 128] = 1.0
    s = np.arange(128)[:, None] % 64
    t = np.arange(64)[None, :]
    c[:, C_MASK:C_MASK + 64] = (s <= t).astype(np.float32)
    rm = np.ones(512, np.float32)
    rm[::64] = 0.0
    c[:, C_RMASK:C_RMASK + 512] = rm[None, :]
    c[:, C_MISC + 0] = EPS
    c[:, C_MISC + 1] = 1.0
    c[:, C_MISC + 2] = 0.0
    c[:, C_MISC + 3] = 0.25
    return c


def fm(v, nchunk):
    v = np.asarray(v, np.float32)
    lead = v.shape[:-1]
    v = v.reshape(lead + (nchunk, 128))
    v = np.moveaxis(v, -1, 0)
    return np.ascontiguousarray(v).reshape(128, -1)


def pack_params(inp):
    p = np.zeros((128, NPRM), np.float32)
    def put(name, arr):
        o = PRM_OFF[name]
        p[:arr.shape[0], o:o + arr.shape[1]] = arr
    put("ff1_norm", fm(inp["ff1_norm"], 8))
    put("mix_norm", fm(inp["mix_norm"], 8))
    put("ff2_norm", fm(inp["ff2_norm"], 8))
    put("final_norm", fm(inp["final_norm"], 8))
    put("m_head_norm", fm(inp["m_head_norm"], 8))
    put("m_b_i", np.asarray(inp["m_b_i"], np.float32).T.copy())
    put("m_b_f", np.asarray(inp["m_b_f"], np.float32).T.copy())
    put("r_conv_w", fm(np.asarray(inp["r_conv_w"], np.float32)[:, :, 0, :], 10))
    put("r_conv_b", fm(inp["r_conv_b"], 10))
    put("r_b_a", fm(inp["r_b_a"], 10))
    put("r_b_i", fm(inp["r_b_i"], 10))
    put("r_lam", fm(inp["r_lam"], 10))
    return p


def slab_plan():
    plan = []
    for L in range(4):
        for which in (1,):
            for s in range(11):
                plan.append(("ffin", L, (1, s), 8 * 512))
            for mp in range(4):
                plan.append(("ffout", L, (1, mp), 22 * 256))
        if L % 2 == 0:
            plan.append(("m_gate", L, 0, 8 * 16))
            for s in (0, 2, 3, 4, 5, 1):
                plan.append(("m_in", L, s, 8 * 512))
            for s in range(2):
                plan.append(("m_out", L, s, 8 * 512))
        else:
            for s in range(5):
                plan.append(("r_in", L, s, 8 * 512))
            for g in range(2):
                plan.append(("r_gate", L, g, 10 * 3 * 128))
            for s in range(2):
                plan.append(("r_out", L, s, 10 * 512))
        for s in range(11):
            plan.append(("ffin", L, (2, s), 8 * 512))
        for mp in range(4):
            plan.append(("ffout", L, (2, mp), 22 * 256))
    return plan


def _kslab(w, cols):
    sub = np.asarray(w)[:, cols]
    C = sub.shape[0] // 128
    return np.ascontiguousarray(sub.reshape(C, 128, sub.shape[1]).transpose(1, 0, 2)).reshape(128, -1)


def pack_wstream(inp):
    plan = slab_plan()
    tot = sum(p[3] for p in plan)
    ws = np.zeros((128, tot), np.float32)
    off = 0
    for kind, L, idx, size in plan:
        j = L // 2
        if kind == "ffin":
            which, s = idx
            w = inp["ff1_w_in" if which == 1 else "ff2_w_in"][L]
            cols = np.r_[s * 256:(s + 1) * 256, DFF + s * 256:DFF + (s + 1) * 256]
            blk = _kslab(w, cols)
        elif kind == "ffout":
            which, mp = idx
            w = inp["ff1_w_out" if which == 1 else "ff2_w_out"][L]
            blk = _kslab(w, np.r_[mp * 256:(mp + 1) * 256])
        elif kind == "m_gate":
            blk = _kslab(inp["m_w_in"][j], np.r_[3072:3088])
        elif kind == "m_in":
            blk = _kslab(inp["m_w_in"][j], np.r_[idx * 512:(idx + 1) * 512])
        elif kind == "m_out":
            blk = _kslab(inp["m_w_out"][j], np.r_[idx * 512:(idx + 1) * 512])
        elif kind == "r_in":
            blk = _kslab(inp["r_w_in"][j], np.r_[idx * 512:(idx + 1) * 512])
        elif kind == "r_out":
            blk = _kslab(inp["r_w_out"][j], np.r_[idx * 512:(idx + 1) * 512])
        elif kind == "r_gate":
            wg = np.asarray(inp["r_w_a" if idx == 0 else "r_w_i"][j], np.float32)
            dense = np.zeros((RW, RW), np.float32)
            for n in range(8):
                dense[n * 160:(n + 1) * 160, n * 160:(n + 1) * 160] = wg[n]
            blk = np.zeros((128, 10, 3, 128), np.float32)
            for jo in range(10):
                for di in range(3):
                    ji = jo + di - 1
                    if 0 <= ji < 10:
                        blk[:, jo, di, :] = dense[ji * 128:(ji + 1) * 128, jo * 128:(jo + 1) * 128]
            blk = blk.reshape(128, -1)
        assert blk.shape[1] == size, (kind, blk.shape, size)
        ws[:, off:off + size] = blk
        off += size
    return ws


class Op:
    __slots__ = ("eng", "fn", "waits", "sig", "ticket", "dma", "dsem", "dval", "idx")


class Prog:
    ENGS = ("pe", "act", "dve", "pool", "sp")

    def __init__(self):
        self.ops = []
        self.eng_ops = {e: [] for e in self.ENGS}
        self.last_w = {}
        self.readers = {}
        self.known = {e: {f: -1 for f in self.ENGS} for e in self.ENGS}
        self.known_dma = {e: set() for e in self.ENGS}
        self.vc = {}
        self.ndma = 0
        self.ndma_sw = 0
        self.dma_last = [None] * NDMASEM
        self.dma_cnt = [0] * NDMASEM
        self.fence_deps = set()
        self.fence_touched = set()

    def fence(self):
        col = set(self.fence_deps)
        for k in [k for k in self.last_w if isinstance(k, tuple) and str(k[0]).startswith("MA:")]:
            col.add(self.last_w.pop(k))
        for k in [k for k in self.readers if isinstance(k, tuple) and str(k[0]).startswith("MA:")]:
            col.update(self.readers.pop(k).values())
        best = {}
        keep = set()
        for d in col:
            o = self.ops[d]
            if o.dma:
                keep.add(d)
            else:
                best[o.eng] = max(best.get(o.eng, -1), d)
        keep.update(best.values())
        self.fence_deps = keep
        self.fence_touched = set()

    def op(self, eng, fn, reads=(), writes=(), dma=False):
        o = Op()
        o.eng, o.fn, o.dma, o.sig, o.ticket = eng, fn, dma, False, None
        o.idx = len(self.ops)
        deps = set()
        if self.fence_deps:
            for k in tuple(reads) + tuple(writes):
                if isinstance(k, tuple) and str(k[0]).startswith("MA:") and k not in self.fence_touched:
                    self.fence_touched.add(k)
                    deps.update(self.fence_deps)
        for k in reads:
            w = self.last_w.get(k)
            if w is not None:
                deps.add(w)
        for k in writes:
            w = self.last_w.get(k)
            if w is not None:
                deps.add(w)
            r = self.readers.get(k)
            if r:
                deps.update(r.values())
        if dma:
            if eng == "pool":
                slot = 8 + self.ndma_sw % (NDMASEM - 8)
                self.ndma_sw += 1
            else:
                slot = self.ndma % 8
                self.ndma += 1
            prev = self.dma_last[slot]
            if prev is not None:
                deps.add(prev)
            self.dma_last[slot] = o.idx
            self.dma_cnt[slot] += 16
            o.dsem, o.dval = slot, self.dma_cnt[slot]
        waits = []
        kn = self.known[eng]
        for d in sorted(deps):
            dop = self.ops[d]
            if dop.dma:
                if d in self.known_dma[eng]:
                    continue
                self.known_dma[eng].add(d)
                waits.append(d)
            else:
                f = dop.eng
                if f == eng:
                    if eng == "pe" or not SAME_ENG_SYNC:
                        continue
                if kn[f] >= d:
                    continue
                dop.sig = True
                waits.append(d)
                for g, v in self.vc[d].items():
                    if v > kn[g]:
                        kn[g] = v
        o.waits = waits
        if not dma:
            v = dict(kn)
            v[eng] = o.idx
            self.vc[o.idx] = v
        for k in reads:
            self.readers.setdefault(k, {})[("d", o.idx) if dma else eng] = o.idx
        for k in writes:
            self.last_w[k] = o.idx
            self.readers[k] = {}
        self.ops.append(o)
        self.eng_ops[eng].append(o)
        return o

    def emit(self, nc, es):
        sems = {e: es.enter_context(nc.semaphore("s_" + e)) for e in self.ENGS}
        dsems = [es.enter_context(nc.semaphore("d_%d" % i)) for i in range(NDMASEM)]
        for e in self.ENGS:
            t = 0
            for o in self.eng_ops[e]:
                if o.sig:
                    t += 1
                    o.ticket = t
            assert t < 60000, (e, t)
        for c in self.dma_cnt:
            assert c < 60000
        block = es.enter_context(nc.Block())
        ops = self.ops

        def run(eng_name):
            def body(eng):
                for o in self.eng_ops[eng_name]:
                    for d in o.waits:
                        dop = ops[d]
                        if dop.dma:
                            eng.wait_ge(dsems[dop.dsem], dop.dval)
                        else:
                            eng.wait_ge(sems[dop.eng], dop.ticket)
                    if o.fn is None:
                        continue
                    ins = o.fn(eng)
                    if o.dma:
                        ins.then_inc(dsems[o.dsem], 16)
                    elif o.sig:
                        ins.then_inc(sems[eng_name], 1)
            return body
        block.tensor(run("pe"))
        block.scalar(run("act"))
        block.vector(run("dve"))
        block.gpsimd(run("pool"))
        block.sync(run("sp"))


def bcast(ap, pos, n):
    pairs = [list(p) for p in ap.ap]
    pairs.insert(pos, [0, n])
    return bass.AP(ap.tensor, ap.offset, pairs)


class Builder:
    def __init__(self, layers=(0, 1, 2, 3), ntiles=8, parts=("f1", "mix", "f2"), final_norm=True):
        self.layers, self.ntiles, self.parts, self.final_norm = tuple(layers), ntiles, parts, final_norm
        self.P = Prog()
        self.plan = [p for p in slab_plan() if p[1] in self.layers and self._part_on(p)]
        self.full_plan = slab_plan()
        offs, o = [], 0
        for p in self.full_plan:
            offs.append(o)
            o += p[3]
        self.wtot = o
        self.plan_off = [offs[i] for i, p in enumerate(self.full_plan) if p[1] in self.layers and self._part_on(p)]
        self.ws_next = 0
        self.ws_issued = 0
        self.ws_total = len(self.plan) * ntiles
        self.pb = 0

    def _part_on(self, p):
        kind = p[0]
        if kind in ("ffin", "ffout"):
            return ("f1" if p[2][0] == 1 else "f2") in self.parts
        return "mix" in self.parts

    def bank(self):
        b = self.pb
        self.pb = (b + 1) % 8
        return b

    def PSb(self, b, rows=slice(0, 128), cols=slice(0, 512)):
        return self.PS[rows, b * 512 + cols.start: b * 512 + cols.stop]

    def mm_group(self, out, pairs, reads, writes, eng="pe"):
        n = len(pairs)
        def fn(pe, out=out, pairs=pairs, n=n):
            ins = None
            for i, (l, r) in enumerate(pairs):
                ins = pe.matmul(out, l, r, start=(i == 0), stop=(i == n - 1))
            return ins
        return self.P.op("pe", fn, reads, writes)

    def mm_chain(self, out, pairs, reads_each, common, writes):
        n = len(pairs)
        for i, (l, r) in enumerate(pairs):
            self.P.op("pe", lambda pe, l=l, r=r, i=i: pe.matmul(out, l, r, start=(i == 0), stop=(i == n - 1)),
                      tuple(reads_each[i]) + tuple(common), writes)

    def mm(self, out, l, r, reads, writes, start=True, stop=True):
        return self.P.op("pe", lambda pe: pe.matmul(out, l, r, start=start, stop=stop), reads, writes)

    def act(self, out, in_, func, reads, writes, bias=None, scale=None, accum_out=None):
        kw = {}
        if bias is not None:
            kw["bias"] = bias
        if scale is not None:
            kw["scale"] = scale
        if accum_out is not None:
            kw["accum_out"] = accum_out
        return self.P.op("act", lambda e: e.activation(out=out, in_=in_, func=func, **kw), reads, writes)

    def tt(self, eng, out, in0, in1, op, reads, writes):
        return self.P.op(eng, lambda e: e.tensor_tensor(out, in0, in1, op), reads, writes)

    def ts(self, eng, out, in0, s1, s2, op0, op1, reads, writes):
        if op1 is None:
            return self.P.op(eng, lambda e: e.tensor_scalar(out, in0, s1, None, op0), reads, writes)
        return self.P.op(eng, lambda e: e.tensor_scalar(out, in0, s1, s2, op0, op1), reads, writes)

    def stt(self, eng, out, in0, s, in1, op0, op1, reads, writes):
        return self.P.op(eng, lambda e: e.scalar_tensor_tensor(out, in0, s, in1, op0, op1), reads, writes)

    def cp(self, eng, out, in_, reads, writes):
        if eng == "act":
            return self.P.op("act", lambda e: e.copy(out, in_), reads, writes)
        return self.P.op(eng, lambda e: e.tensor_copy(out, in_), reads, writes)

    def ws_issue(self, k):
        n = len(self.plan)
        pi = k % n
        size = self.plan[pi][3]
        off = self.plan_off[pi]
        b = k % NB
        dst = self.WB[b][:, 0:size]
        src = self.wstream[:, off:off + size]
        self.P.op("pool", lambda g: g.dma_start(out=dst, in_=src), reads=(), writes=(("W", b),), dma=True)

    def ws_get(self, kind):
        k = self.ws_next
        self.ws_next += 1
        while self.ws_issued < min(k + NB - 1, self.ws_total):
            self.ws_issue(self.ws_issued)
            self.ws_issued += 1
        p = self.plan[k % len(self.plan)]
        assert p[0] == kind, (p, kind)
        return k % NB

    def wview(self, b, C, ncols):
        return self.WB[b][:, 0:C * ncols].rearrange("p (c n) -> p c n", c=C)

    def build(self):
        nc = bass.Bass("TRN2", target_bir_lowering=False)
        self.nc = nc
        ntok = self.ntiles * T
        self.x_d = nc.dram_tensor("x", [ntok, D], F32, kind="ExternalInput").ap()
        self.wstream = nc.dram_tensor("wstream", [128, self.wtot], F32, kind="ExternalInput").ap()
        self.prm_d = nc.dram_tensor("prm", [128, NPRM], F32, kind="ExternalInput").ap()
        self.cst_d = nc.dram_tensor("cst", [128, NCST], F32, kind="ExternalInput").ap()
        self.y_d = nc.dram_tensor("y", [ntok, D], F32, kind="ExternalOutput").ap()
        with ExitStack() as es:
            def sb(name, shape, dt):
                return es.enter_context(nc.sbuf_tensor(name, shape, dt))
            self.X = sb("X", [128, NCH, T], F32)
            self.XN = sb("XN", [128, NCH, T], BF16)
            self.AR = sb("AR", [128, 24, T], BF16)
            self.WB = [sb("WB%d" % i, [128, SLABW], BF16) for i in range(NB)]
            self.MA = sb("MA", [128, 15400], F32)
            self.PRM = sb("PRM", [128, NPRM], F32)
            self.CST = sb("CST", [128, NCST], F32)
            self.IDB = sb("IDB", [128, 128], BF16)
            self.ONESB = sb("ONESB", [128, 128], BF16)
            self.RS = sb("RS", [128, T], F32)
            self.DUM = sb("DUM", [128, 2], F32)
            self.SG = sb("SG", [128, 2, T], F32)
            self.VEXT = sb("VEXT", [64, 8, HEADS, VW], BF16)
            self.KTOK = sb("KTOK", [64, 8, 512], BF16)
            self.SPT = sb("SPT", [64, 2, 512], BF16)
            self.CSTATE = sb("CSTATE", [64, 2, HEADS, VW], F32)
            self.MST = sb("MST", [8, 2, 12], F32)
            self.RTAIL = sb("RTAIL", [128, 2, RC, 4], F32)
            self.RH = sb("RH", [128, 2, RC], F32)
            self.RSC = sb("RSC", [128, 2, 2, RC], F32)
            self.RHB = sb("RHB", [128, 2, 2, RC], F32)
            self.PS = es.enter_context(nc.psum_tensor("PS", [128, 4096], F32))
            self.record()
            self.P.emit(nc, es)
        return nc

    def record(self):
        P = self.P
        CST, PRM = self.CST, self.PRM
        P.op("sp", lambda q: q.dma_start(out=CST[:], in_=self.cst_d[:, :]), (), ("CST",), dma=True)
        P.op("sp", lambda q: q.dma_start(out=PRM[:], in_=self.prm_d[:, :]), (), ("PRM",), dma=True)
        self.ID = CST[:, C_ID:C_ID + 128]
        self.ONES = CST[:, C_ONES:C_ONES + 128]
        self.MASK = CST[:, C_MASK:C_MASK + 64]
        self.RMASK = CST[0:8, C_RMASK:C_RMASK + 512]
        self.c_eps = CST[:, C_MISC:C_MISC + 1]
        self.c_one = CST[:, C_MISC + 1:C_MISC + 2]
        self.c_zero = CST[:, C_MISC + 2:C_MISC + 3]
        self.c_quarter = CST[:, C_MISC + 3:C_MISC + 4]
        self.cp("dve", self.IDB[:], self.ID, ("CST",), ("IDB",))
        self.cp("dve", self.ONESB[:], self.ONES, ("CST",), ("ONESB",))
        P.op("dve", lambda e: e.memset(self.CSTATE[:], 0.0), (), (("CSTATE", 0), ("CSTATE", 1)))
        P.op("dve", lambda e: e.memset(self.MST[:], 0.0), (), (("MST", 0), ("MST", 1)))
        P.op("dve", lambda e: e.memset(self.RTAIL[:], 0.0), (), (("RTAIL", 0), ("RTAIL", 1)))
        P.op("dve", lambda e: e.memset(self.RH[:], 0.0), (), tuple(("RH", a, b) for a in range(2) for b in range(RC)))
        P.op("dve", lambda e: e.memset(self.VEXT[:], 1.0), (), tuple(("VEXT", i) for i in range(8)))
        if "mix" in self.parts and any(L % 2 for L in self.layers):
            self.rglru_consts()
        for ti in range(self.ntiles):
            self.load_x(ti)
            for L in self.layers:
                if "f1" in self.parts:
                    self.ffn(L, 1)
                if "mix" in self.parts:
                    if L % 2 == 0:
                        self.mlstm(L)
                    else:
                        self.rglru(L)
                if "f2" in self.parts:
                    self.ffn(L, 2)
            self.store_x(ti)
        P.op("sp", None, tuple(("YOUT", i) for i in range(4)), ())

    def load_x(self, ti):
        P = self.P
        P.fence()
        XS = self.MA[:, 0:4096].rearrange("p (b n) -> p b n", b=4)
        for tb in range(4):
            src = self.x_d[ti * T + tb * 128: ti * T + (tb + 1) * 128, :]
            dst = XS[:, tb, :]
            P.op("sp", lambda q, dst=dst, src=src: q.dma_start(out=dst, in_=src), (), (("MA:IO", tb),), dma=True)
        for c in range(NCH):
            b = self.bank()
            for tb in range(4):
                self.mm(self.PSb(b, cols=slice(tb * 128, (tb + 1) * 128)), XS[:, tb, c * 128:(c + 1) * 128], self.ID,
                        (("MA:IO", tb), "CST"), (("P", b),))
            self.cp("act" if c % 2 else "dve", self.X[:, c, :], self.PSb(b), (("P", b),), (("X", c),))

    def store_x(self, ti):
        P = self.P
        P.fence()
        if self.final_norm:
            self.norm(PRM_OFF["final_norm"], out_f32=True)
            src_key = "XF"
        OS = self.MA[:, 0:4096].rearrange("p (b n) -> p b n", b=4)
        XF = self.MA[:, 4096:8192].rearrange("p (c n) -> p c n", c=8)
        for tb in range(4):
            for half in range(2):
                b = self.bank()
                for cc in range(4):
                    c = half * 4 + cc
                    src = XF[:, c, tb * 128:(tb + 1) * 128] if self.final_norm else self.X[:, c, tb * 128:(tb + 1) * 128]
                    rk = ("MA:IO", 4 + c // 2) if self.final_norm else ("X", c)
                    self.mm(self.PSb(b, cols=slice(cc * 128, (cc + 1) * 128)), src, self.ID, (rk, "CST"), (("P", b),))
                self.cp("act" if half else "dve", OS[:, tb, half * 512:(half + 1) * 512], self.PSb(b), (("P", b),), (("MA:IO", tb),))
            dst = self.y_d[ti * T + tb * 128: ti * T + (tb + 1) * 128, :]
            src = OS[:, tb, :]
            P.op("sp", lambda q, dst=dst, src=src: q.dma_start(out=dst, in_=src), (("MA:IO", tb),), (("YOUT", tb),), dma=True)

    def norm(self, goff, out_f32=False):
        b = self.bank()
        self.act(self.DUM[:, 0:1], self.c_one, AF.Sqrt, ("CST",), ("DUM",))
        for c in range(NCH):
            self.act(self.AR[:, c, :], self.X[:, c, :], AF.Square, (("X", c),), (("A", c),))
        self.mm_chain(self.PSb(b), [(self.ONESB[:], self.AR[:, c, :]) for c in range(NCH)],
                      [(("A", c),) for c in range(NCH)], ("ONESB",), (("P", b),))
        self.act(self.RS[:], self.PSb(b), AF.Sqrt, (("P", b), "CST"), ("RS",), bias=self.c_eps, scale=1.0 / D)
        self.P.op("dve", lambda e: e.reciprocal(self.RS[:], self.RS[:]), ("RS",), ("RS",))
        XF = self.MA[:, 4096:8192].rearrange("p (c n) -> p c n", c=8)
        for c in range(NCH):
            g = self.PRM[:, goff + c: goff + c + 1]
            if out_f32:
                self.stt("dve", XF[:, c, :], self.X[:, c, :], g, self.RS[:], ALU.mult, ALU.mult,
                         (("X", c), "RS", "PRM"), (("MA:IO", 4 + c // 2),))
            else:
                self.stt("dve", self.XN[:, c, :], self.X[:, c, :], g, self.RS[:], ALU.mult, ALU.mult,
                         (("X", c), "RS", "PRM"), (("XN", c),))

    def ffn(self, L, which):
        self.norm(PRM_OFF["ff1_norm" if which == 1 else "ff2_norm"] + L * 8)
        xn_keys = tuple(("XN", c) for c in range(NCH))
        for s in range(11):
            wb = self.ws_get("ffin")
            W = self.wview(wb, 8, 512)
            for jj in range(2):
                j = 2 * s + jj
                bg, bu = self.bank(), self.bank()
                if j == 0:
                    self.mm_chain(self.PSb(bg), [(W[:, c, jj * 128:(jj + 1) * 128], self.XN[:, c, :]) for c in range(NCH)],
                                  [(("XN", c),) for c in range(NCH)], (("W", wb),), (("P", bg),))
                else:
                    self.mm_group(self.PSb(bg), [(W[:, c, jj * 128:(jj + 1) * 128], self.XN[:, c, :]) for c in range(NCH)],
                                  xn_keys + (("W", wb),), (("P", bg),))
                self.mm_group(self.PSb(bu), [(W[:, c, 256 + jj * 128:256 + (jj + 1) * 128], self.XN[:, c, :]) for c in range(NCH)],
                              xn_keys + (("W", wb),), (("P", bu),))
                self.act(self.SG[:, j % 2, :], self.PSb(bg), AF.Silu, (("P", bg),), (("SG", j % 2),))
                self.tt("dve", self.AR[:, j, :], self.SG[:, j % 2, :], self.PSb(bu), ALU.mult,
                        (("SG", j % 2), ("P", bu)), (("A", j),))
        hm_keys = tuple(("A", j) for j in range(NJ))
        for mp in range(4):
            wb = self.ws_get("ffout")
            W = self.wview(wb, 22, 256)
            for mm_ in range(2):
                m = 2 * mp + mm_
                b = self.bank()
                if m == 0:
                    prs = [(W[:, j, mm_ * 128:(mm_ + 1) * 128], self.AR[:, j, :]) for j in range(NJ)]
                    def f1(pe, prs=prs, o=self.PSb(b)):
                        ins = None
                        for i in range(18):
                            ins = pe.matmul(o, prs[i][0], prs[i][1], start=(i == 0), stop=False)
                        return ins
                    self.P.op("pe", f1, tuple(("A", j) for j in range(18)) + (("W", wb),), (("P", b),))
                    for i in range(18, NJ):
                        self.P.op("pe", lambda pe, i=i, prs=prs, o=self.PSb(b): pe.matmul(o, prs[i][0], prs[i][1], start=False, stop=(i == NJ - 1)),
                                  (("A", i), ("W", wb)), (("P", b),))
                else:
                    self.mm_group(self.PSb(b), [(W[:, j, mm_ * 128:(mm_ + 1) * 128], self.AR[:, j, :]) for j in range(NJ)],
                                  hm_keys + (("W", wb),), (("P", b),))
                self.stt("dve", self.X[:, m, :], self.PSb(b), 0.5, self.X[:, m, :], ALU.mult, ALU.add,
                         (("P", b), ("X", m)), (("X", m),))

    def rglru_consts(self):
        o = PRM_OFF["r_lam"]
        lam = self.PRM[:, o:o + 20]
        r0 = self.RSC[:, 0].rearrange("p l c -> p (l c)")
        self.act(r0, lam, AF.Exp, ("PRM",), ("RSC",), scale=-1.0)
        self.act(r0, r0, AF.Ln, ("RSC", "CST"), ("RSC",), bias=self.c_one)
        self.ts("dve", r0, r0, -4.0, None, ALU.mult, None, ("RSC",), ("RSC",))
        for g, nm in ((0, "r_b_a"), (1, "r_b_i")):
            self.ts("dve", self.RHB[:, g].rearrange("p l c -> p (l c)"), self.PRM[:, PRM_OFF[nm]:PRM_OFF[nm] + 20], 0.5, None, ALU.mult, None,
                    ("PRM",), ("RHB",))

    def rglru(self, L):
        P = self.P
        j = L // 2
        self.norm(PRM_OFF["mix_norm"] + L * 8)
        P.fence()
        MA = self.MA
        UB = MA[:, 0:5160].rearrange("p (c n) -> p c n", c=RC)
        XC = MA[:, 5160:10280].rearrange("p (c n) -> p c n", c=RC)
        def SC(st, i):
            o = 10280 + (st * 5 + i) * 512
            return MA[:, o:o + 512]
        xn_keys = tuple(("XN", c) for c in range(NCH))
        self.cp("dve", UB[:, :, 0:4], self.RTAIL[:, j], (("RTAIL", j),), (("MA:UBT",),))
        for s_ in range(5):
            wb = self.ws_get("r_in")
            W = self.wview(wb, 8, 512)
            for qq in range(4):
                q = s_ * 4 + qq
                b = self.bank()
                self.mm_group(self.PSb(b), [(W[:, c, qq * 128:(qq + 1) * 128], self.XN[:, c, :]) for c in range(NCH)],
                              xn_keys + (("W", wb),), (("P", b),))
                if q < RC:
                    self.cp("act", UB[:, q, 4:516], self.PSb(b), (("P", b),), (("MA:UB", q),))
                else:
                    self.act(self.AR[:, q - RC, :], self.PSb(b), AF.Gelu_apprx_tanh, (("P", b),), (("A", q - RC),))
        ocw = PRM_OFF["r_conv_w"] + j * 40
        ocb = PRM_OFF["r_conv_b"] + j * 10
        def cwap(kk, jc):
            return self.PRM[:, ocw + kk * 10 + jc: ocw + kk * 10 + jc + 1]
        for jp in range(0, RC, 2):
            for jc in (jp, jp + 1):
                self.act(XC[:, jc, :], UB[:, jc, 1:513], AF.Identity, (("MA:UB", jc), ("MA:UBT",), "PRM"), (("MA:XC", jc),),
                         bias=self.PRM[:, ocb + jc: ocb + jc + 1], scale=cwap(0, jc))
            for kk in range(1, 4):
                for jc in (jp, jp + 1):
                    self.stt("dve", XC[:, jc, :], UB[:, jc, 1 + kk:513 + kk], cwap(kk, jc), XC[:, jc, :], ALU.mult, ALU.add,
                             (("MA:UB", jc), ("MA:UBT",), ("MA:XC", jc), "PRM"), (("MA:XC", jc),))
            for jc in (jp, jp + 1):
                self.cp("pool", self.AR[:, 10 + jc, :], XC[:, jc, :], (("MA:XC", jc),), (("A", 10 + jc),))
        self.cp("dve", self.RTAIL[:, j], UB[:, :, 512:516], tuple(("MA:UB", jc) for jc in range(RC)), (("RTAIL", j),))
        P.fence()
        wa = self.ws_get("r_gate")
        wi = self.ws_get("r_gate")
        WA = self.WB[wa][:, 0:3840].rearrange("p (j d n) -> p j d n", j=RC, d=3)
        WI = self.WB[wi][:, 0:3840].rearrange("p (j d n) -> p j d n", j=RC, d=3)
        def SCR(st, i):
            n = st * 3 + i
            o = 10280 + n * 512 if n < 10 else (n - 10) * 512
            return MA[:, o:o + 512]
        hsc = lambda jo: self.RSC[:, 0, j, jo:jo + 1]
        KA = lambda jo: ("MA:SC", jo % 6, 0)
        KB = lambda jo: ("MA:SC", jo % 6, 1)
        KC = lambda jo: ("MA:SC", jo % 6, 2)
        BA = lambda jo: SCR(jo % 6, 0)
        BB = lambda jo: SCR(jo % 6, 1)
        BC = lambda jo: SCR(jo % 6, 2)

        def stA(jos):
            banks = {}
            for jo in jos:
                dis = [di for di in range(3) if 0 <= jo + di - 1 < RC]
                rk = tuple(("A", 10 + jo + di - 1) for di in dis)
                ba, bi = self.bank(), self.bank()
                banks[jo] = (ba, bi)
                self.mm_group(self.PSb(ba), [(WA[:, jo, di, :], self.AR[:, 10 + jo + di - 1, :]) for di in dis],
                              rk + (("W", wa),), (("P", ba),))
                self.mm_group(self.PSb(bi), [(WI[:, jo, di, :], self.AR[:, 10 + jo + di - 1, :]) for di in dis],
                              rk + (("W", wi),), (("P", bi),))
            for jo in jos:
                ba, bi = banks[jo]
                self.act(BA(jo), self.PSb(ba), AF.Tanh, (("P", ba), "RHB"), (KA(jo),), bias=self.RHB[:, 0, j, jo:jo + 1], scale=0.5)
                self.act(BB(jo), self.PSb(bi), AF.Tanh, (("P", bi), "RHB"), (KB(jo),), bias=self.RHB[:, 1, j, jo:jo + 1], scale=0.5)
            for jo in jos:
                self.act(BA(jo), BA(jo), AF.Identity, (KA(jo), "RSC"), (KA(jo),), bias=hsc(jo), scale=hsc(jo))

        def stB(jos):
            for jo in jos:
                self.ts("pool", BC(jo), BA(jo), 1.0 / 6.0, 0.5, ALU.mult, ALU.add, (KA(jo),), (KC(jo),))
            for jo in jos:
                self.tt("pool", BC(jo), BC(jo), BA(jo), ALU.mult, (KC(jo), KA(jo)), (KC(jo),))
            for jo in jos:
                self.stt("dve", BC(jo), BC(jo), 1.0, BA(jo), ALU.add, ALU.mult, (KC(jo), KA(jo)), (KC(jo),))
            for jo in jos:
                self.ts("pool", BA(jo), BC(jo), 1.0, None, ALU.add, None, (KC(jo),), (KA(jo),))
            for jo in jos:
                self.stt("dve", BB(jo), BB(jo), 1.0, XC[:, jo, :], ALU.add, ALU.mult, (KB(jo), ("MA:XC", jo)), (KB(jo),))
            for jo in jos:
                self.act(BC(jo), BA(jo), AF.Square, (KA(jo),), (KC(jo),))

        def stC(jos):
            for jo in jos:
                self.act(BC(jo), BC(jo), AF.Sqrt, (KC(jo), "CST"), (KC(jo),), bias=self.c_quarter, scale=-0.25)
            for jo in jos:
                self.tt("dve", BB(jo), BB(jo), BC(jo), ALU.mult, (KB(jo), KC(jo)), (KB(jo),))
            for jo in jos:
                hinit = self.RH[:, j, jo:jo + 1]
                P.op("dve", lambda e, o=BC(jo), a_=BA(jo), b_=BB(jo), hinit=hinit: e.tensor_tensor_scan(o, a_, b_, hinit, ALU.mult, ALU.add),
                     (KA(jo), KB(jo), ("RH", j, jo)), (KC(jo),))
            for jo in jos:
                self.cp("dve", self.RH[:, j, jo:jo + 1], BC(jo)[:, 511:512], (KC(jo),), (("RH", j, jo),))
                self.tt("dve", self.AR[:, jo, :], BC(jo), self.AR[:, jo, :], ALU.mult, (KC(jo), ("A", jo)), (("A", jo),))

        NP_ = RC // 2
        for it in range(NP_ + 2):
            if it < NP_:
                stA((2 * it, 2 * it + 1))
            if 1 <= it <= NP_:
                stB((2 * it - 2, 2 * it - 1))
            if 2 <= it <= NP_ + 1:
                stC((2 * it - 4, 2 * it - 3))
        yk = tuple(("A", jr) for jr in range(RC))
        for s_ in range(2):
            wb = self.ws_get("r_out")
            W = self.wview(wb, RC, 512)
            for mm_ in range(4):
                m = s_ * 4 + mm_
                b = self.bank()
                self.mm_group(self.PSb(b), [(W[:, jr, mm_ * 128:(mm_ + 1) * 128], self.AR[:, jr, :]) for jr in range(RC)],
                              yk + (("W", wb),), (("P", b),))
                self.tt("dve", self.X[:, m, :], self.X[:, m, :], self.PSb(b), ALU.add, (("P", b), ("X", m)), (("X", m),))

    def mlstm(self, L):
        P = self.P
        j = L // 2
        self.norm(PRM_OFF["mix_norm"] + L * 8)
        P.fence()
        MA, AR, PRM, PS = self.MA, self.AR, self.PRM, self.PS
        GI = MA[0:8, 0:512]
        NLF = MA[0:8, 512:1024]
        NBC = MA[0:8, 1024:1536]
        EF = MA[0:8, 1536:2048]
        FF = MA[0:8, 2048:2560]
        TMP8 = MA[0:8, 2560:3072]
        A8 = MA[0:8, 3072:3080]
        M8 = MA[0:8, 3080:3088]
        W8 = MA[0:8, 3088:3096]
        NEGBF = MA[0:8, 3096:3097]
        WD = MA[0:8, 3136:3200]
        ETW = MA[0:64, 3200:3392]
        ETOK = MA[:, 3200:3328].rearrange("p (c n) -> p c n", c=8)
        WBC = MA[:, 3328:3392].rearrange("p (c h) -> p c h", c=8)
        STMPS = [MA[:, 3392:3904], MA[:, 3904:4416]]
        DENS = [MA[:, 4416:4480], MA[:, 4480:4544]]
        TMPC = MA[0:64, 4544:5600].rearrange("p (h n) -> p h n", h=8)
        NUMS = [MA[:, 5632:6688], MA[:, 6688:7744]]
        SQ = MA[:, 7744:8768]
        xn_keys = tuple(("XN", c) for c in range(NCH))
        k = lambda n, *a: ("MA:" + n,) + a
        marr = self.MST[0:8, j, :]
        mk = ("MST", j)
        wb = self.ws_get("m_gate")
        WG = self.wview(wb, 8, 16)
        bi_, bf_ = self.bank(), self.bank()
        self.mm_group(self.PSb(bi_, rows=slice(0, 8)), [(WG[:, c, 0:8], self.XN[:, c, :]) for c in range(NCH)],
                      xn_keys + (("W", wb),), (("P", bi_),))
        self.mm_group(self.PSb(bf_, rows=slice(0, 8)), [(WG[:, c, 8:16], self.XN[:, c, :]) for c in range(NCH)],
                      xn_keys + (("W", wb),), (("P", bf_),))
        obi = PRM_OFF["m_b_i"] + j
        obf = PRM_OFF["m_b_f"] + j
        self.ts("dve", NEGBF, PRM[0:8, obf:obf + 1], -1.0, None, ALU.mult, None, ("PRM",), (k("NEGBF"),))
        self.act(NLF, self.PSb(bf_, rows=slice(0, 8)), AF.Exp, (("P", bf_), k("NEGBF")), (k("NLF"),), bias=NEGBF, scale=-1.0)
        self.act(NLF, NLF, AF.Ln, (k("NLF"), "CST"), (k("NLF"),), bias=self.c_one[0:8])
        P.op("dve", lambda e: e.tensor_tensor_scan(NBC, self.RMASK, NLF, 0.0, ALU.mult, ALU.add),
             (k("NLF"), "CST"), (k("NBC"),))
        self.stt("dve", GI, self.PSb(bi_, rows=slice(0, 8)), PRM[0:8, obi:obi + 1], NBC, ALU.add, ALU.add,
                 (("P", bi_), "PRM", k("NBC")), (k("GI"),))
        GI3 = GI.rearrange("p (c t) -> p c t", c=8)
        NBC3 = NBC.rearrange("p (c t) -> p c t", c=8)
        P.op("dve", lambda e: e.tensor_reduce(A8, GI3, AX.X, ALU.max), (k("GI"),), (k("A8"),))
        for c in range(8):
            self.tt("dve", M8[:, c:c + 1], marr[:, c:c + 1], A8[:, c:c + 1], ALU.max, (mk, k("A8")), (k("M8"),))
            self.tt("dve", marr[:, c + 1:c + 2], M8[:, c:c + 1], NBC3[:, c, 63:64], ALU.subtract, (k("M8"), k("NBC")), (mk,))
        self.tt("dve", W8, marr[:, 0:8], M8, ALU.subtract, (mk, k("M8")), (k("W8"),))
        self.act(W8, W8, AF.Exp, (k("W8"),), (k("W8"),))
        self.cp("dve", marr[:, 0:1], marr[:, 8:9], (mk, k("W8")), (mk,))
        M8b = bcast(M8, 2, 64)
        self.tt("dve", TMP8.rearrange("p (c t) -> p c t", c=8), GI3, M8b, ALU.subtract, (k("GI"), k("M8")), (k("TMP8"),))
        self.act(EF, TMP8, AF.Exp, (k("TMP8"),), (k("EF"),))
        self.tt("dve", FF.rearrange("p (c t) -> p c t", c=8), NBC3, M8b, ALU.subtract, (k("NBC"), k("M8")), (k("FF"),))
        self.act(FF, FF, AF.Exp, (k("FF"),), (k("FF"),))
        self.tt("dve", WD.rearrange("p (c h) -> p c h", c=8), bcast(W8, 2, 8), bcast(self.CST[0:8, C_ID:C_ID + 8], 1, 8), ALU.mult,
                (k("W8"), "CST"), (k("WD"),))
        MAB = MA[:, 8768:15400].bitcast(BF16)
        QK = MAB[:, 0:8192].rearrange("p (b n) -> p b n", b=16)
        HNBS = [MAB[:, 8192:9216], MAB[:, 9216:10240]]
        CBFS = [MAB[:, 10240 + i * 8 * VW: 10240 + (i + 1) * 8 * VW].rearrange("p (h n) -> p h n", h=8) for i in range(2)]
        R64 = slice(0, 64)
        wb = self.ws_get("m_in")
        W = self.wview(wb, 8, 512)
        for h in range(8):
            b = self.bank()
            self.mm_group(self.PSb(b, rows=R64), [(W[:, c, h * 64:(h + 1) * 64], self.XN[:, c, :]) for c in range(NCH)],
                          xn_keys + (("W", wb),), (("P", b),))
            P.op("act", lambda e, o=QK[R64, h, :], i=self.PSb(b, rows=R64): e.mul(o, i, 0.125), (("P", b),), (k("QK", h),))
        for vs in range(2):
            wb = self.ws_get("m_in")
            W = self.wview(wb, 8, 512)
            for c in range(8):
                b = self.bank()
                self.mm_group(self.PSb(b, rows=R64), [(self.XN[:, c_, c * 64:(c + 1) * 64], W[:, c_, :]) for c_ in range(NCH)],
                              xn_keys + (("W", wb),), (("P", b),))
                self.cp("act", self.VEXT[R64, c, vs * 4:(vs + 1) * 4, 0:128], self.PSb(b, rows=R64).rearrange("p (h v) -> p h v", h=4),
                        (("P", b),), (("VEXT", c),))
        for os_ in range(2):
            wb = self.ws_get("m_in")
            W = self.wview(wb, 8, 512)
            for jo in range(4):
                h = os_ * 4 + jo
                b = self.bank()
                self.mm_group(self.PSb(b), [(W[:, c, jo * 128:(jo + 1) * 128], self.XN[:, c, :]) for c in range(NCH)],
                              xn_keys + (("W", wb),), (("P", b),))
                self.act(AR[:, 8 + h, :], self.PSb(b), AF.Sigmoid, (("P", b),), (("A", 8 + h),))
                P.op("act", lambda e, o=AR[:, 8 + h, :], w=PRM[:, PRM_OFF["m_head_norm"] + j * 8 + h: PRM_OFF["m_head_norm"] + j * 8 + h + 1]: e.mul(o, o, w),
                     (("A", 8 + h), "PRM"), (("A", 8 + h),))
        bt_ = self.bank()
        I8 = self.CST[0:8, C_ID:C_ID + 8]
        for c in range(8):
            self.mm(self.PSb(bt_, rows=slice(0, 64), cols=slice(c * 16, c * 16 + 8)), EF[:, c * 64:(c + 1) * 64], I8,
                    (k("EF"), "CST"), (("P", bt_),))
            self.mm(self.PSb(bt_, rows=slice(0, 64), cols=slice(c * 16 + 8, c * 16 + 16)), FF[:, c * 64:(c + 1) * 64], I8,
                    (k("FF"), "CST"), (("P", bt_),))
        self.mm(self.PSb(bt_, rows=slice(0, 64), cols=slice(128, 192)), self.CST[0:8, C_ONES:C_ONES + 64], WD, (k("WD"), "CST"), (("P", bt_),))
        self.cp("dve", ETW, self.PSb(bt_, rows=slice(0, 64), cols=slice(0, 192)), (("P", bt_),), (k("ETW"),))
        wb = self.ws_get("m_in")
        W = self.wview(wb, 8, 512)
        for h in range(8):
            b = self.bank()
            self.mm_group(self.PSb(b, rows=R64), [(W[:, c, h * 64:(h + 1) * 64], self.XN[:, c, :]) for c in range(NCH)],
                          xn_keys + (("W", wb),), (("P", b),))
            self.cp("act", QK[R64, 8 + h, :], self.PSb(b, rows=R64), (("P", b),), (k("QK", 8 + h),))
        for c in range(8):
            b = self.bank()
            self.mm_group(self.PSb(b, rows=R64), [(self.XN[:, c_, c * 64:(c + 1) * 64], W[:, c_, :]) for c_ in range(NCH)],
                          xn_keys + (("W", wb),), (("P", b),))
            self.tt("dve", self.KTOK[R64, c, :].rearrange("p (h d) -> p h d", h=8),
                    self.PSb(b, rows=R64).rearrange("p (h d) -> p h d", h=8), bcast(ETOK[R64, c, 0:8], 2, 64), ALU.mult,
                    (("P", b), k("ETW")), (("KTOK", c),))
        CS = self.CSTATE
        csk = ("CSTATE", j)
        qk = tuple(k("QK", i) for i in range(16))
        pk = (("P", 4), ("P", 5), ("P", 6))
        dk = (("P", 1), ("P", 2), ("P", 3))
        def PD(h):
            o = 512 * (1 + h // 3) + (h % 3) * VW
            return PS[R64, o:o + 129]
        def PN(h):
            o = 512 * (4 + h // 3) + (h % 3) * VW
            return PS[R64, o:o + 129]
        def bankview(base, bnk):
            nh = 3 if bnk < 2 else 2
            return PS[R64, 512 * (base + bnk): 512 * (base + bnk) + nh * VW].rearrange("p (h n) -> p h n", h=nh), nh
        import os
        NCK = int(os.environ.get('DBG_NCHUNK', '8'))
        SIGH = AR[:, 8:16, :]
        YT = AR[:, 16:24, :]

        def S1a(c):
            tc0 = c * 64
            def fS(pe, tc0=tc0):
                ins = None
                for h in range(8):
                    ins = pe.matmul(PS[R64, h * 64:(h + 1) * 64], QK[R64, 8 + h, tc0:tc0 + 64], QK[R64, h, tc0:tc0 + 64], start=True, stop=True)
                return ins
            P.op("pe", fS, qk, (("P", 0),))

        def S1b(c):
            pc = c % 2
            self.tt("dve", STMPS[pc][R64, :].rearrange("p (h t) -> p h t", h=8), PS[R64, 0:512].rearrange("p (h t) -> p h t", h=8),
                    bcast(ETOK[R64, c, 0:8], 2, 64), ALU.mult, (("P", 0), k("ETW")), (k("STMP", pc),))
            self.tt("pool", self.SPT[R64, pc, :].rearrange("p (h t) -> p h t", h=8), STMPS[pc][R64, :].rearrange("p (h t) -> p h t", h=8),
                    bcast(self.MASK[R64, :], 1, 8), ALU.mult, (k("STMP", pc), "CST"), (("SPT", pc),))

        def S1(c):
            S1a(c)
            S1b(c)

        def evac(c):
            tc0 = c * 64
            self.tt("dve", YT[:, :, tc0:tc0 + 64], PS[:, 3584:4096].rearrange("p (h t) -> p h t", h=8), SIGH[:, :, tc0:tc0 + 64], ALU.mult,
                    (("P", 7),) + tuple(("A", 8 + h) for h in range(8)), tuple(("A", 16 + h) for h in range(8)))

        def fTop(c):
            HNB = HNBS[c % 2]
            def fT(pe, HNB=HNB):
                ins = None
                for h in range(8):
                    ins = pe.matmul(PS[:, 3584 + h * 64: 3584 + (h + 1) * 64], HNB[R64, h * 128:(h + 1) * 128], self.IDB[R64, 0:64], start=True, stop=True)
                return ins
            P.op("pe", fT, (k("HN", c % 2), "IDB"), (("P", 7),))

        S1(0)
        for c in range(NCK):
            pc, tc0 = c % 2, c * 64
            CBF, NUM, DEN, HNB = CBFS[pc], NUMS[pc], DENS[pc], HNBS[pc]
            NUM3 = NUM[R64, :].rearrange("p (h n) -> p h n", h=8)
            SQ3 = SQ[R64, :].rearrange("p (h v) -> p h v", h=8)
            d0, d1 = DEN[R64, 0:8], DEN[R64, 8:16]
            self.tt("dve", TMPC, CS[R64, j, :, :], bcast(WBC[R64, c, :], 2, VW), ALU.mult, (csk, k("ETW")), (k("TMPC"),))
            self.cp("act", CBF[R64, :, :], TMPC, (k("TMPC"),), (k("CBF", pc),))
            def fD(pe, c=c):
                ins = None
                for h in range(8):
                    ins = pe.matmul(PD(h), self.KTOK[R64, c, h * 64:(h + 1) * 64], self.VEXT[R64, c, h, 0:129], start=True, stop=True)
                return ins
            P.op("pe", fD, (("KTOK", c), ("VEXT", c)), dk)
            if c + 1 < NCK:
                S1a(c + 1)
            for bnk in range(3):
                v, nh = bankview(1, bnk)
                self.tt("dve", CS[R64, j, 3 * bnk:3 * bnk + nh, 0:129], TMPC[:, 3 * bnk:3 * bnk + nh, 0:129], v[:, :, 0:129], ALU.add,
                        (k("TMPC"), ("P", 1 + bnk)), (csk,))
            if c + 1 < NCK:
                S1b(c + 1)
            def fN(pe, c=c, tc0=tc0, pc=pc, CBF=CBF):
                ins = None
                for h in range(8):
                    pe.matmul(PN(h), self.SPT[R64, pc, h * 64:(h + 1) * 64], self.VEXT[R64, c, h, 0:129], start=True, stop=False)
                    ins = pe.matmul(PN(h), QK[R64, h, tc0:tc0 + 64], CBF[R64, h, 0:129], start=False, stop=True)
                return ins
            P.op("pe", fN, (("SPT", pc), ("VEXT", c), k("CBF", pc)) + qk, pk)
            if c > 0:
                fTop(c - 1)
            for bnk in range(3):
                v, nh = bankview(4, bnk)
                self.cp("act", NUM3[:, 3 * bnk:3 * bnk + nh, 0:129], v[:, :, 0:129], (("P", 4 + bnk),), (k("NUM", pc),))
            self.act(d0, NUM3[:, :, 128], AF.Abs, (k("NUM", pc),), (k("D0", pc),))
            if c > 0:
                evac(c - 1)
            self.tt("dve", d0, d0, ETOK[R64, c, 8:16], ALU.max, (k("D0", pc), k("ETW")), (k("D0", pc),))
            self.act(d0, d0, AF.Square, (k("D0", pc),), (k("D0", pc),), scale=float(np.sqrt(EPS)))
            self.tt("pool", SQ3, NUM3[:, :, 0:128], NUM3[:, :, 0:128], ALU.mult, (k("NUM", pc),), (k("SQ"),))
            P.op("dve", lambda e, d1=d1, SQ3=SQ3: e.tensor_reduce(d1, SQ3, AX.X, ALU.add), (k("SQ"),), (k("D1", pc),))
            self.stt("dve", d1, d1, 1.0 / 128.0, d0, ALU.mult, ALU.add, (k("D1", pc), k("D0", pc)), (k("D1", pc),))
            self.act(d1, d1, AF.Sqrt, (k("D1", pc),), (k("D1", pc),))
            P.op("dve", lambda e, d1=d1: e.reciprocal(d1, d1), (k("D1", pc),), (k("D1", pc),))
            self.tt("pool", HNB[R64, :].rearrange("p (h v) -> p h v", h=8), NUM3[:, :, 0:128], bcast(d1, 2, 128), ALU.mult,
                    (k("NUM", pc), k("D1", pc)), (k("HN", pc),))
        if NCK > 0:
            fTop(NCK - 1)
            evac(NCK - 1)
        yk = tuple(("A", 16 + h) for h in range(8))
        for s_ in range(2 if int(os.environ.get('DBG_NCHUNK', '8')) == 8 else 0):
            wb = self.ws_get("m_out")
            W = self.wview(wb, 8, 512)
            for mm_ in range(4):
                m = s_ * 4 + mm_
                b = self.bank()
                self.mm_group(self.PSb(b), [(W[:, h, mm_ * 128:(mm_ + 1) * 128], AR[:, 16 + h, :]) for h in range(8)],
                              yk + (("W", wb),), (("P", b),))
                self.tt("dve", self.X[:, m, :], self.X[:, m, :], self.PSb(b), ALU.add, (("P", b), ("X", m)), (("X", m),))


_CACHE = {}


def _get_nc(key, **kw):
    if key not in _CACHE:
        b = Builder(**kw)
        _CACHE[key] = (b, b.build())
    return _CACHE[key]


def kernel(**inputs):
    inp = {k: np.asarray(v) for k, v in inputs.items()}
    x = inp["x"]
    B = x.shape[0]
    b, nc = _get_nc("full")
    ws = pack_wstream(inp)
    prm = pack_params(inp)
    cst = pack_consts()
    in_maps = [{"x": np.ascontiguousarray(x[i]), "wstream": ws, "prm": prm, "cst": cst} for i in range(B)]
    res = run_bass_kernel_spmd(nc, in_maps, core_ids=list(range(B)))
    return np.stack([np.asarray(r["y"]) for r in res.results], axis=0).astype(np.float32)
```

```python
import numpy as np
from contextlib import ExitStack
import concourse.bass as bass
import concourse.mybir as mybir
from concourse.bass_utils import run_bass_kernel_spmd

F32 = mybir.dt.float32
BF16 = mybir.dt.bfloat16
AF = mybir.ActivationFunctionType
ALU = mybir.AluOpType
AX = mybir.AxisListType

D = 1024
DFF = 2816
SEQ = 4096
T = 512
NCH = 8
NJ = 22
HEADS = 8
RW = 1280
RC = 10
EPS = 1e-6
NB = 4
SLABW = 5632
VW = 132
NDMASEM = 24
SAME_ENG_SYNC = True

PRM_OFF = {}
def _prm_layout():
    off = 0
    for name, n in (("ff1_norm", 32), ("mix_norm", 32), ("ff2_norm", 32), ("final_norm", 8),
                    ("m_head_norm", 16), ("m_b_i", 2), ("m_b_f", 2), ("r_conv_w", 80),
                    ("r_conv_b", 20), ("r_b_a", 20), ("r_b_i", 20), ("r_lam", 20)):
        PRM_OFF[name] = off
        off += n
    return off
NPRM = _prm_layout()

C_ID, C_ONES, C_MASK, C_RMASK, C_MISC = 0, 128, 256, 320, 832
NCST = 848


def pack_consts():
    c = np.zeros((128, NCST), np.float32)
    c[:, C_ID:C_ID + 128] = np.eye(128, dtype=np.float32)
    c[:, C_ONES:C_ONES + 128] = 1.0
    s = np.arange(128)[:, None] % 64
    t = np.arange(64)[None, :]
    c[:, C_MASK:C_MASK + 64] = (s <= t).astype(np.float32)
    rm = np.ones(512, np.float32)
    rm[::64] = 0.0
    c[:, C_RMASK:C_RMASK + 512] = rm[None, :]
    c[:, C_MISC + 0] = EPS
    c[:, C_MISC + 1] = 1.0
    c[:, C_MISC + 2] = 0.0
    c[:, C_MISC + 3] = 0.25
    return c


def fm(v, nchunk):
    v = np.asarray(v, np.float32)
    lead = v.shape[:-1]
    v = v.reshape(lead + (nchunk, 128))
    v = np.moveaxis(v, -1, 0)
    return np.ascontiguousarray(v).reshape(128, -1)


def pack_params(inp):
    p = np.zeros((128, NPRM), np.float32)
    def put(name, arr):
        o = PRM_OFF[name]
        p[:arr.shape[0], o:o + arr.shape[1]] = arr
    put("ff1_norm", fm(inp["ff1_norm"], 8))
    put("mix_norm", fm(inp["mix_norm"], 8))
    put("ff2_norm", fm(inp["ff2_norm"], 8))
    put("final_norm", fm(inp["final_norm"], 8))
    put("m_head_norm", fm(inp["m_head_norm"], 8))
    put("m_b_i", np.asarray(inp["m_b_i"], np.float32).T.copy())
    put("m_b_f", np.asarray(inp["m_b_f"], np.float32).T.copy())
    put("r_conv_w", fm(np.asarray(inp["r_conv_w"], np.float32)[:, :, 0, :], 10))
    put("r_conv_b", fm(inp["r_conv_b"], 10))
    put("r_b_a", fm(inp["r_b_a"], 10))
    put("r_b_i", fm(inp["r_b_i"], 10))
    put("r_lam", fm(inp["r_lam"], 10))
    return p


def slab_plan():
    plan = []
    for L in range(4):
        for which in (1,):
            for s in range(11):
                plan.append(("ffin", L, (1, s), 8 * 512))
            for mp in range(4):
                plan.append(("ffout", L, (1, mp), 22 * 256))
        if L % 2 == 0:
            plan.append(("m_gate", L, 0, 8 * 16))
            for s in (0, 2, 3, 4, 5, 1):
                plan.append(("m_in", L, s, 8 * 512))
            for s in range(2):
                plan.append(("m_out", L, s, 8 * 512))
        else:
            for s in range(5):
                plan.append(("r_in", L, s, 8 * 512))
            for g in range(2):
                plan.append(("r_gate", L, g, 10 * 3 * 128))
            for s in range(2):
                plan.append(("r_out", L, s, 10 * 512))
        for s in range(11):
            plan.append(("ffin", L, (2, s), 8 * 512))
        for mp in range(4):
            plan.append(("ffout", L, (2, mp), 22 * 256))
    return plan


def _kslab(w, cols):
    sub = np.asarray(w)[:, cols]
    C = sub.shape[0] // 128
    return np.ascontiguousarray(sub.reshape(C, 128, sub.shape[1]).transpose(1, 0, 2)).reshape(128, -1)


def pack_wstream(inp):
    plan = slab_plan()
    tot = sum(p[3] for p in plan)
    ws = np.zeros((128, tot), np.float32)
    off = 0
    for kind, L, idx, size in plan:
        j = L // 2
        if kind == "ffin":
            which, s = idx
            w = inp["ff1_w_in" if which == 1 else "ff2_w_in"][L]
            cols = np.r_[s * 256:(s + 1) * 256, DFF + s * 256:DFF + (s + 1) * 256]
            blk = _kslab(w, cols)
        elif kind == "ffout":
            which, mp = idx
            w = inp["ff1_w_out" if which == 1 else "ff2_w_out"][L]
            blk = _kslab(w, np.r_[mp * 256:(mp + 1) * 256])
        elif kind == "m_gate":
            blk = _kslab(inp["m_w_in"][j], np.r_[3072:3088])
        elif kind == "m_in":
            blk = _kslab(inp["m_w_in"][j], np.r_[idx * 512:(idx + 1) * 512])
        elif kind == "m_out":
            blk = _kslab(inp["m_w_out"][j], np.r_[idx * 512:(idx + 1) * 512])
        elif kind == "r_in":
            blk = _kslab(inp["r_w_in"][j], np.r_[idx * 512:(idx + 1) * 512])
        elif kind == "r_out":
            blk = _kslab(inp["r_w_out"][j], np.r_[idx * 512:(idx + 1) * 512])
        elif kind == "r_gate":
            wg = np.asarray(inp["r_w_a" if idx == 0 else "r_w_i"][j], np.float32)
            dense = np.zeros((RW, RW), np.float32)
            for n in range(8):
                dense[n * 160:(n + 1) * 160, n * 160:(n + 1) * 160] = wg[n]
            blk = np.zeros((128, 10, 3, 128), np.float32)
            for jo in range(10):
                for di in range(3):
                    ji = jo + di - 1
                    if 0 <= ji < 10:
                        blk[:, jo, di, :] = dense[ji * 128:(ji + 1) * 128, jo * 128:(jo + 1) * 128]
            blk = blk.reshape(128, -1)
        assert blk.shape[1] == size, (kind, blk.shape, size)
        ws[:, off:off + size] = blk
        off += size
    return ws


class Op:
    __slots__ = ("eng", "fn", "waits", "sig", "ticket", "dma", "dsem", "dval", "idx")


class Prog:
    ENGS = ("pe", "act", "dve", "pool", "sp")

    def __init__(self):
        self.ops = []
        self.eng_ops = {e: [] for e in self.ENGS}
        self.last_w = {}
        self.readers = {}
        self.known = {e: {f: -1 for f in self.ENGS} for e in self.ENGS}
        self.known_dma = {e: set() for e in self.ENGS}
        self.vc = {}
        self.ndma = 0
        self.ndma_sw = 0
        self.dma_last = [None] * NDMASEM
        self.dma_cnt = [0] * NDMASEM
        self.fence_deps = set()
        self.fence_touched = set()

    def fence(self):
        col = set(self.fence_deps)
        for k in [k for k in self.last_w if isinstance(k, tuple) and str(k[0]).startswith("MA:")]:
            col.add(self.last_w.pop(k))
        for k in [k for k in self.readers if isinstance(k, tuple) and str(k[0]).startswith("MA:")]:
            col.update(self.readers.pop(k).values())
        best = {}
        keep = set()
        for d in col:
            o = self.ops[d]
            if o.dma:
                keep.add(d)
            else:
                best[o.eng] = max(best.get(o.eng, -1), d)
        keep.update(best.values())
        self.fence_deps = keep
        self.fence_touched = set()

    def op(self, eng, fn, reads=(), writes=(), dma=False):
        o = Op()
        o.eng, o.fn, o.dma, o.sig, o.ticket = eng, fn, dma, False, None
        o.idx = len(self.ops)
        deps = set()
        if self.fence_deps:
            for k in tuple(reads) + tuple(writes):
                if isinstance(k, tuple) and str(k[0]).startswith("MA:") and k not in self.fence_touched:
                    self.fence_touched.add(k)
                    deps.update(self.fence_deps)
        for k in reads:
            w = self.last_w.get(k)
            if w is not None:
                deps.add(w)
        for k in writes:
            w = self.last_w.get(k)
            if w is not None:
                deps.add(w)
            r = self.readers.get(k)
            if r:
                deps.update(r.values())
        if dma:
            if eng == "pool":
                slot = 8 + self.ndma_sw % (NDMASEM - 8)
                self.ndma_sw += 1
            else:
                slot = self.ndma % 8
                self.ndma += 1
            prev = self.dma_last[slot]
            if prev is not None:
                deps.add(prev)
            self.dma_last[slot] = o.idx
            self.dma_cnt[slot] += 16
            o.dsem, o.dval = slot, self.dma_cnt[slot]
        waits = []
        kn = self.known[eng]
        for d in sorted(deps):
            dop = self.ops[d]
            if dop.dma:
                if d in self.known_dma[eng]:
                    continue
                self.known_dma[eng].add(d)
                waits.append(d)
            else:
                f = dop.eng
                if f == eng:
                    if eng == "pe" or not SAME_ENG_SYNC:
                        continue
                if kn[f] >= d:
                    continue
                dop.sig = True
                waits.append(d)
                for g, v in self.vc[d].items():
                    if v > kn[g]:
                        kn[g] = v
        o.waits = waits
        if not dma:
            v = dict(kn)
            v[eng] = o.idx
            self.vc[o.idx] = v
        for k in reads:
            self.readers.setdefault(k, {})[("d", o.idx) if dma else eng] = o.idx
        for k in writes:
            self.last_w[k] = o.idx
            self.readers[k] = {}
        self.ops.append(o)
        self.eng_ops[eng].append(o)
        return o

    def emit(self, nc, es):
        sems = {e: es.enter_context(nc.semaphore("s_" + e)) for e in self.ENGS}
        dsems = [es.enter_context(nc.semaphore("d_%d" % i)) for i in range(NDMASEM)]
        for e in self.ENGS:
            t = 0
            for o in self.eng_ops[e]:
                if o.sig:
                    t += 1
                    o.ticket = t
            assert t < 60000, (e, t)
        for c in self.dma_cnt:
            assert c < 60000
        block = es.enter_context(nc.Block())
        ops = self.ops

        def run(eng_name):
            def body(eng):
                for o in self.eng_ops[eng_name]:
                    for d in o.waits:
                        dop = ops[d]
                        if dop.dma:
                            eng.wait_ge(dsems[dop.dsem], dop.dval)
                        else:
                            eng.wait_ge(sems[dop.eng], dop.ticket)
                    if o.fn is None:
                        continue
                    ins = o.fn(eng)
                    if o.dma:
                        ins.then_inc(dsems[o.dsem], 16)
                    elif o.sig:
                        ins.then_inc(sems[eng_name], 1)
            return body
        block.tensor(run("pe"))
        block.scalar(run("act"))
        block.vector(run("dve"))
        block.gpsimd(run("pool"))
        block.sync(run("sp"))


def bcast(ap, pos, n):
    pairs = [list(p) for p in ap.ap]
    pairs.insert(pos, [0, n])
    return bass.AP(ap.tensor, ap.offset, pairs)


class Builder:
    def __init__(self, layers=(0, 1, 2, 3), ntiles=8, parts=("f1", "mix", "f2"), final_norm=True):
        self.layers, self.ntiles, self.parts, self.final_norm = tuple(layers), ntiles, parts, final_norm
        self.P = Prog()
        self.plan = [p for p in slab_plan() if p[1] in self.layers and self._part_on(p)]
        self.full_plan = slab_plan()
        offs, o = [], 0
        for p in self.full_plan:
            offs.append(o)
            o += p[3]
        self.wtot = o
        self.plan_off = [offs[i] for i, p in enumerate(self.full_plan) if p[1] in self.layers and self._part_on(p)]
        self.ws_next = 0
        self.ws_issued = 0
        self.ws_total = len(self.plan) * ntiles
        self.pb = 0

    def _part_on(self, p):
        kind = p[0]
        if kind in ("ffin", "ffout"):
            return ("f1" if p[2][0] == 1 else "f2") in self.parts
        return "mix" in self.parts

    def bank(self):
        b = self.pb
        self.pb = (b + 1) % 8
        return b

    def PSb(self, b, rows=slice(0, 128), cols=slice(0, 512)):
        return self.PS[rows, b * 512 + cols.start: b * 512 + cols.stop]

    def mm_group(self, out, pairs, reads, writes, eng="pe"):
        n = len(pairs)
        def fn(pe, out=out, pairs=pairs, n=n):
            ins = None
            for i, (l, r) in enumerate(pairs):
                ins = pe.matmul(out, l, r, start=(i == 0), stop=(i == n - 1))
            return ins
        return self.P.op("pe", fn, reads, writes)

    def mm_chain(self, out, pairs, reads_each, common, writes):
        n = len(pairs)
        for i, (l, r) in enumerate(pairs):
            self.P.op("pe", lambda pe, l=l, r=r, i=i: pe.matmul(out, l, r, start=(i == 0), stop=(i == n - 1)),
                      tuple(reads_each[i]) + tuple(common), writes)

    def mm(self, out, l, r, reads, writes, start=True, stop=True):
        return self.P.op("pe", lambda pe: pe.matmul(out, l, r, start=start, stop=stop), reads, writes)

    def act(self, out, in_, func, reads, writes, bias=None, scale=None, accum_out=None):
        kw = {}
        if bias is not None:
            kw["bias"] = bias
        if scale is not None:
            kw["scale"] = scale
        if accum_out is not None:
            kw["accum_out"] = accum_out
        return self.P.op("act", lambda e: e.activation(out=out, in_=in_, func=func, **kw), reads, writes)

    def tt(self, eng, out, in0, in1, op, reads, writes):
        return self.P.op(eng, lambda e: e.tensor_tensor(out, in0, in1, op), reads, writes)

    def ts(self, eng, out, in0, s1, s2, op0, op1, reads, writes):
        if op1 is None:
            return self.P.op(eng, lambda e: e.tensor_scalar(out, in0, s1, None, op0), reads, writes)
        return self.P.op(eng, lambda e: e.tensor_scalar(out, in0, s1, s2, op0, op1), reads, writes)

    def stt(self, eng, out, in0, s, in1, op0, op1, reads, writes):
        return self.P.op(eng, lambda e: e.scalar_tensor_tensor(out, in0, s, in1, op0, op1), reads, writes)

    def cp(self, eng, out, in_, reads, writes):
        if eng == "act":
            return self.P.op("act", lambda e: e.copy(out, in_), reads, writes)
        return self.P.op(eng, lambda e: e.tensor_copy(out, in_), reads, writes)

    def ws_issue(self, k):
        n = len(self.plan)
        pi = k % n
        size = self.plan[pi][3]
        off = self.plan_off[pi]
        b = k % NB
        dst = self.WB[b][:, 0:size]
        src = self.wstream[:, off:off + size]
        self.P.op("pool", lambda g: g.dma_start(out=dst, in_=src), reads=(), writes=(("W", b),), dma=True)

    def ws_get(self, kind):
        k = self.ws_next
        self.ws_next += 1
        while self.ws_issued < min(k + NB - 1, self.ws_total):
            self.ws_issue(self.ws_issued)
            self.ws_issued += 1
        p = self.plan[k % len(self.plan)]
        assert p[0] == kind, (p, kind)
        return k % NB

    def wview(self, b, C, ncols):
        return self.WB[b][:, 0:C * ncols].rearrange("p (c n) -> p c n", c=C)

    def build(self):
        nc = bass.Bass("TRN2", target_bir_lowering=False)
        self.nc = nc
        ntok = self.ntiles * T
        self.x_d = nc.dram_tensor("x", [ntok, D], F32, kind="ExternalInput").ap()
        self.wstream = nc.dram_tensor("wstream", [128, self.wtot], F32, kind="ExternalInput").ap()
        self.prm_d = nc.dram_tensor("prm", [128, NPRM], F32, kind="ExternalInput").ap()
        self.cst_d = nc.dram_tensor("cst", [128, NCST], F32, kind="ExternalInput").ap()
        self.y_d = nc.dram_tensor("y", [ntok, D], F32, kind="ExternalOutput").ap()
        with ExitStack() as es:
            def sb(name, shape, dt):
                return es.enter_context(nc.sbuf_tensor(name, shape, dt))
            self.X = sb("X", [128, NCH, T], F32)
            self.XN = sb("XN", [128, NCH, T], BF16)
            self.AR = sb("AR", [128, 24, T], BF16)
            self.WB = [sb("WB%d" % i, [128, SLABW], BF16) for i in range(NB)]
            self.MA = sb("MA", [128, 15400], F32)
            self.PRM = sb("PRM", [128, NPRM], F32)
            self.CST = sb("CST", [128, NCST], F32)
            self.IDB = sb("IDB", [128, 128], BF16)
            self.ONESB = sb("ONESB", [128, 128], BF16)
            self.RS = sb("RS", [128, T], F32)
            self.DUM = sb("DUM", [128, 2], F32)
            self.SG = sb("SG", [128, 2, T], F32)
            self.VEXT = sb("VEXT", [64, 8, HEADS, VW], BF16)
            self.KTOK = sb("KTOK", [64, 8, 512], BF16)
            self.SPT = sb("SPT", [64, 2, 512], BF16)
            self.CSTATE = sb("CSTATE", [64, 2, HEADS, VW], F32)
            self.MST = sb("MST", [8, 2, 12], F32)
            self.RTAIL = sb("RTAIL", [128, 2, RC, 4], F32)
            self.RH = sb("RH", [128, 2, RC], F32)
            self.RSC = sb("RSC", [128, 2, 2, RC], F32)
            self.RHB = sb("RHB", [128, 2, 2, RC], F32)
            self.PS = es.enter_context(nc.psum_tensor("PS", [128, 4096], F32))
            self.record()
            self.P.emit(nc, es)
        return nc

    def record(self):
        P = self.P
        CST, PRM = self.CST, self.PRM
        P.op("sp", lambda q: q.dma_start(out=CST[:], in_=self.cst_d[:, :]), (), ("CST",), dma=True)
        P.op("sp", lambda q: q.dma_start(out=PRM[:], in_=self.prm_d[:, :]), (), ("PRM",), dma=True)
        self.ID = CST[:, C_ID:C_ID + 128]
        self.ONES = CST[:, C_ONES:C_ONES + 128]
        self.MASK = CST[:, C_MASK:C_MASK + 64]
        self.RMASK = CST[0:8, C_RMASK:C_RMASK + 512]
        self.c_eps = CST[:, C_MISC:C_MISC + 1]
        self.c_one = CST[:, C_MISC + 1:C_MISC + 2]
        self.c_zero = CST[:, C_MISC + 2:C_MISC + 3]
        self.c_quarter = CST[:, C_MISC + 3:C_MISC + 4]
        self.cp("dve", self.IDB[:], self.ID, ("CST",), ("IDB",))
        self.cp("dve", self.ONESB[:], self.ONES, ("CST",), ("ONESB",))
        P.op("dve", lambda e: e.memset(self.CSTATE[:], 0.0), (), (("CSTATE", 0), ("CSTATE", 1)))
        P.op("dve", lambda e: e.memset(self.MST[:], 0.0), (), (("MST", 0), ("MST", 1)))
        P.op("dve", lambda e: e.memset(self.RTAIL[:], 0.0), (), (("RTAIL", 0), ("RTAIL", 1)))
        P.op("dve", lambda e: e.memset(self.RH[:], 0.0), (), tuple(("RH", a, b) for a in range(2) for b in range(RC)))
        P.op("dve", lambda e: e.memset(self.VEXT[:], 1.0), (), tuple(("VEXT", i) for i in range(8)))
        if "mix" in self.parts and any(L % 2 for L in self.layers):
            self.rglru_consts()
        for ti in range(self.ntiles):
            self.load_x(ti)
            for L in self.layers:
                if "f1" in self.parts:
                    self.ffn(L, 1)
                if "mix" in self.parts:
                    if L % 2 == 0:
                        self.mlstm(L)
                    else:
                        self.rglru(L)
                if "f2" in self.parts:
                    self.ffn(L, 2)
            self.store_x(ti)
        P.op("sp", None, tuple(("YOUT", i) for i in range(4)), ())

    def load_x(self, ti):
        P = self.P
        P.fence()
        XS = self.MA[:, 0:4096].rearrange("p (b n) -> p b n", b=4)
        for tb in range(4):
            src = self.x_d[ti * T + tb * 128: ti * T + (tb + 1) * 128, :]
            dst = XS[:, tb, :]
            P.op("sp", lambda q, dst=dst, src=src: q.dma_start(out=dst, in_=src), (), (("MA:IO", tb),), dma=True)
        for c in range(NCH):
            b = self.bank()
            for tb in range(4):
                self.mm(self.PSb(b, cols=slice(tb * 128, (tb + 1) * 128)), XS[:, tb, c * 128:(c + 1) * 128], self.ID,
                        (("MA:IO", tb), "CST"), (("P", b),))
            self.cp("act" if c % 2 else "dve", self.X[:, c, :], self.PSb(b), (("P", b),), (("X", c),))

    def store_x(self, ti):
        P = self.P
        P.fence()
        if self.final_norm:
            self.norm(PRM_OFF["final_norm"], out_f32=True)
            src_key = "XF"
        OS = self.MA[:, 0:4096].rearrange("p (b n) -> p b n", b=4)
        XF = self.MA[:, 4096:8192].rearrange("p (c n) -> p c n", c=8)
        for tb in range(4):
            for half in range(2):
                b = self.bank()
                for cc in range(4):
                    c = half * 4 + cc
                    src = XF[:, c, tb * 128:(tb + 1) * 128] if self.final_norm else self.X[:, c, tb * 128:(tb + 1) * 128]
                    rk = ("MA:IO", 4 + c // 2) if self.final_norm else ("X", c)
                    self.mm(self.PSb(b, cols=slice(cc * 128, (cc + 1) * 128)), src, self.ID, (rk, "CST"), (("P", b),))
                self.cp("act" if half else "dve", OS[:, tb, half * 512:(half + 1) * 512], self.PSb(b), (("P", b),), (("MA:IO", tb),))
            dst = self.y_d[ti * T + tb * 128: ti * T + (tb + 1) * 128, :]
            src = OS[:, tb, :]
            P.op("sp", lambda q, dst=dst, src=src: q.dma_start(out=dst, in_=src), (("MA:IO", tb),), (("YOUT", tb),), dma=True)

    def norm(self, goff, out_f32=False):
        b = self.bank()
        self.act(self.DUM[:, 0:1], self.c_one, AF.Sqrt, ("CST",), ("DUM",))
        for c in range(NCH):
            self.act(self.AR[:, c, :], self.X[:, c, :], AF.Square, (("X", c),), (("A", c),))
        self.mm_chain(self.PSb(b), [(self.ONESB[:], self.AR[:, c, :]) for c in range(NCH)],
                      [(("A", c),) for c in range(NCH)], ("ONESB",), (("P", b),))
        self.act(self.RS[:], self.PSb(b), AF.Sqrt, (("P", b), "CST"), ("RS",), bias=self.c_eps, scale=1.0 / D)
        self.P.op("dve", lambda e: e.reciprocal(self.RS[:], self.RS[:]), ("RS",), ("RS",))
        XF = self.MA[:, 4096:8192].rearrange("p (c n) -> p c n", c=8)
        for c in range(NCH):
            g = self.PRM[:, goff + c: goff + c + 1]
            if out_f32:
                self.stt("dve", XF[:, c, :], self.X[:, c, :], g, self.RS[:], ALU.mult, ALU.mult,
                         (("X", c), "RS", "PRM"), (("MA:IO", 4 + c // 2),))
            else:
                self.stt("dve", self.XN[:, c, :], self.X[:, c, :], g, self.RS[:], ALU.mult, ALU.mult,
                         (("X", c), "RS", "PRM"), (("XN", c),))

    def ffn(self, L, which):
        self.norm(PRM_OFF["ff1_norm" if which == 1 else "ff2_norm"] + L * 8)
        xn_keys = tuple(("XN", c) for c in range(NCH))
        for s in range(11):
            wb = self.ws_get("ffin")
            W = self.wview(wb, 8, 512)
            for jj in range(2):
                j = 2 * s + jj
                bg, bu = self.bank(), self.bank()
                if j == 0:
                    self.mm_chain(self.PSb(bg), [(W[:, c, jj * 128:(jj + 1) * 128], self.XN[:, c, :]) for c in range(NCH)],
                                  [(("XN", c),) for c in range(NCH)], (("W", wb),), (("P", bg),))
                else:
                    self.mm_group(self.PSb(bg), [(W[:, c, jj * 128:(jj + 1) * 128], self.XN[:, c, :]) for c in range(NCH)],
                                  xn_keys + (("W", wb),), (("P", bg),))
                self.mm_group(self.PSb(bu), [(W[:, c, 256 + jj * 128:256 + (jj + 1) * 128], self.XN[:, c, :]) for c in range(NCH)],
                              xn_keys + (("W", wb),), (("P", bu),))
                self.act(self.SG[:, j % 2, :], self.PSb(bg), AF.Silu, (("P", bg),), (("SG", j % 2),))
                self.tt("dve", self.AR[:, j, :], self.SG[:, j % 2, :], self.PSb(bu), ALU.mult,
                        (("SG", j % 2), ("P", bu)), (("A", j),))
        hm_keys = tuple(("A", j) for j in range(NJ))
        NH = 18
        for mp in range(4):
            wb = self.ws_get("ffout")
            W = self.wview(wb, 22, 256)
            if mp == 0:
                bs = [self.bank(), self.bank()]
                prs = [[(W[:, j, mm_ * 128:(mm_ + 1) * 128], self.AR[:, j, :]) for j in range(NJ)] for mm_ in range(2)]
                for mm_ in range(2):
                    def f1(pe, prs=prs[mm_], o=self.PSb(bs[mm_])):
                        ins = None
                        for i in range(NH):
                            ins = pe.matmul(o, prs[i][0], prs[i][1], start=(i == 0), stop=False)
                        return ins
                    self.P.op("pe", f1, tuple(("A", j) for j in range(NH)) + (("W", wb),), (("P", bs[mm_]),))
                for mm_ in range(2):
                    for i in range(NH, NJ):
                        self.P.op("pe", lambda pe, i=i, prs=prs[mm_], o=self.PSb(bs[mm_]): pe.matmul(o, prs[i][0], prs[i][1], start=False, stop=(i == NJ - 1)),
                                  (("A", i), ("W", wb)), (("P", bs[mm_]),))
                    self.stt("dve", self.X[:, mm_, :], self.PSb(bs[mm_]), 0.5, self.X[:, mm_, :], ALU.mult, ALU.add,
                             (("P", bs[mm_]), ("X", mm_)), (("X", mm_),))
                continue
            for mm_ in range(2):
                m = 2 * mp + mm_
                b = self.bank()
                self.mm_group(self.PSb(b), [(W[:, j, mm_ * 128:(mm_ + 1) * 128], self.AR[:, j, :]) for j in range(NJ)],
                              hm_keys + (("W", wb),), (("P", b),))
                self.stt("dve", self.X[:, m, :], self.PSb(b), 0.5, self.X[:, m, :], ALU.mult, ALU.add,
                         (("P", b), ("X", m)), (("X", m),))

    def rglru_consts(self):
        o = PRM_OFF["r_lam"]
        lam = self.PRM[:, o:o + 20]
        r0 = self.RSC[:, 0].rearrange("p l c -> p (l c)")
        self.act(r0, lam, AF.Exp, ("PRM",), ("RSC",), scale=-1.0)
        self.act(r0, r0, AF.Ln, ("RSC", "CST"), ("RSC",), bias=self.c_one)
        self.ts("dve", r0, r0, -4.0, None, ALU.mult, None, ("RSC",), ("RSC",))
        for g, nm in ((0, "r_b_a"), (1, "r_b_i")):
            self.ts("dve", self.RHB[:, g].rearrange("p l c -> p (l c)"), self.PRM[:, PRM_OFF[nm]:PRM_OFF[nm] + 20], 0.5, None, ALU.mult, None,
                    ("PRM",), ("RHB",))

    def rglru(self, L):
        P = self.P
        j = L // 2
        self.norm(PRM_OFF["mix_norm"] + L * 8)
        P.fence()
        MA = self.MA
        UB = MA[:, 0:5160].rearrange("p (c n) -> p c n", c=RC)
        XC = MA[:, 5160:10280].rearrange("p (c n) -> p c n", c=RC)
        def SC(st, i):
            o = 10280 + (st * 5 + i) * 512
            return MA[:, o:o + 512]
        xn_keys = tuple(("XN", c) for c in range(NCH))
        self.cp("dve", UB[:, :, 0:4], self.RTAIL[:, j], (("RTAIL", j),), (("MA:UBT",),))
        for s_ in range(5):
            wb = self.ws_get("r_in")
            W = self.wview(wb, 8, 512)
            for qq in range(4):
                q = s_ * 4 + qq
                b = self.bank()
                self.mm_group(self.PSb(b), [(W[:, c, qq * 128:(qq + 1) * 128], self.XN[:, c, :]) for c in range(NCH)],
                              xn_keys + (("W", wb),), (("P", b),))
                if q < RC:
                    self.cp("act", UB[:, q, 4:516], self.PSb(b), (("P", b),), (("MA:UB", q),))
                else:
                    self.act(self.AR[:, q - RC, :], self.PSb(b), AF.Gelu_apprx_tanh, (("P", b),), (("A", q - RC),))
        ocw = PRM_OFF["r_conv_w"] + j * 40
        ocb = PRM_OFF["r_conv_b"] + j * 10
        def cwap(kk, jc):
            return self.PRM[:, ocw + kk * 10 + jc: ocw + kk * 10 + jc + 1]
        for jp in range(0, RC, 2):
            for jc in (jp, jp + 1):
                self.act(XC[:, jc, :], UB[:, jc, 1:513], AF.Identity, (("MA:UB", jc), ("MA:UBT",), "PRM"), (("MA:XC", jc),),
                         bias=self.PRM[:, ocb + jc: ocb + jc + 1], scale=cwap(0, jc))
            for kk in range(1, 4):
                for jc in (jp, jp + 1):
                    self.stt("dve", XC[:, jc, :], UB[:, jc, 1 + kk:513 + kk], cwap(kk, jc), XC[:, jc, :], ALU.mult, ALU.add,
                             (("MA:UB", jc), ("MA:UBT",), ("MA:XC", jc), "PRM"), (("MA:XC", jc),))
            for jc in (jp, jp + 1):
                self.ts("pool", self.AR[:, 10 + jc, :], XC[:, jc, :], 1.0, 0.0, ALU.mult, ALU.add, (("MA:XC", jc),), (("A", 10 + jc),))
        self.cp("dve", self.RTAIL[:, j], UB[:, :, 512:516], tuple(("MA:UB", jc) for jc in range(RC)), (("RTAIL", j),))
        P.fence()
        wa = self.ws_get("r_gate")
        wi = self.ws_get("r_gate")
        WA = self.WB[wa][:, 0:3840].rearrange("p (j d n) -> p j d n", j=RC, d=3)
        WI = self.WB[wi][:, 0:3840].rearrange("p (j d n) -> p j d n", j=RC, d=3)
        def SCR(st, i):
            n = st * 3 + i
            o = 10280 + n * 512 if n < 10 else (n - 10) * 512
            return MA[:, o:o + 512]
        hsc = lambda jo: self.RSC[:, 0, j, jo:jo + 1]
        KA = lambda jo: ("MA:SC", jo % 6, 0)
        KB = lambda jo: ("MA:SC", jo % 6, 1)
        KC = lambda jo: ("MA:SC", jo % 6, 2)
        BA = lambda jo: SCR(jo % 6, 0)
        BB = lambda jo: SCR(jo % 6, 1)
        BC = lambda jo: SCR(jo % 6, 2)

        def stA(jos):
            banks = {}
            for jo in jos:
                dis = [di for di in range(3) if 0 <= jo + di - 1 < RC]
                rk = tuple(("A", 10 + jo + di - 1) for di in dis)
                ba, bi = self.bank(), self.bank()
                banks[jo] = (ba, bi)
                self.mm_group(self.PSb(ba), [(WA[:, jo, di, :], self.AR[:, 10 + jo + di - 1, :]) for di in dis],
                              rk + (("W", wa),), (("P", ba),))
                self.mm_group(self.PSb(bi), [(WI[:, jo, di, :], self.AR[:, 10 + jo + di - 1, :]) for di in dis],
                              rk + (("W", wi),), (("P", bi),))
            for jo in jos:
                ba, bi = banks[jo]
                self.act(BA(jo), self.PSb(ba), AF.Tanh, (("P", ba), "RHB"), (KA(jo),), bias=self.RHB[:, 0, j, jo:jo + 1], scale=0.5)
                self.act(BB(jo), self.PSb(bi), AF.Tanh, (("P", bi), "RHB"), (KB(jo),), bias=self.RHB[:, 1, j, jo:jo + 1], scale=0.5)
            for jo in jos:
                self.act(BA(jo), BA(jo), AF.Identity, (KA(jo), "RSC"), (KA(jo),), bias=hsc(jo), scale=hsc(jo))

        def stB(jos):
            for jo in jos:
                self.ts("pool", BC(jo), BA(jo), 1.0 / 6.0, 0.5, ALU.mult, ALU.add, (KA(jo),), (KC(jo),))
            for jo in jos:
                self.tt("pool", BC(jo), BC(jo), BA(jo), ALU.mult, (KC(jo), KA(jo)), (KC(jo),))
            for jo in jos:
                self.stt("dve", BC(jo), BC(jo), 1.0, BA(jo), ALU.add, ALU.mult, (KC(jo), KA(jo)), (KC(jo),))
            for jo in jos:
                self.ts("pool", BA(jo), BC(jo), 1.0, 1.0, ALU.mult, ALU.add, (KC(jo),), (KA(jo),))
            for jo in jos:
                self.stt("dve", BB(jo), BB(jo), 1.0, XC[:, jo, :], ALU.add, ALU.mult, (KB(jo), ("MA:XC", jo)), (KB(jo),))
            for jo in jos:
                self.act(BC(jo), BA(jo), AF.Square, (KA(jo),), (KC(jo),))

        def stC(jos):
            for jo in jos:
                self.act(BC(jo), BC(jo), AF.Sqrt, (KC(jo), "CST"), (KC(jo),), bias=self.c_quarter, scale=-0.25)
            for jo in jos:
                self.tt("dve", BB(jo), BB(jo), BC(jo), ALU.mult, (KB(jo), KC(jo)), (KB(jo),))
            for jo in jos:
                hinit = self.RH[:, j, jo:jo + 1]
                P.op("dve", lambda e, o=BC(jo), a_=BA(jo), b_=BB(jo), hinit=hinit: e.tensor_tensor_scan(o, a_, b_, hinit, ALU.mult, ALU.add),
                     (KA(jo), KB(jo), ("RH", j, jo)), (KC(jo),))
            for jo in jos:
                self.cp("dve", self.RH[:, j, jo:jo + 1], BC(jo)[:, 511:512], (KC(jo),), (("RH", j, jo),))
                self.tt("dve", self.AR[:, jo, :], BC(jo), self.AR[:, jo, :], ALU.mult, (KC(jo), ("A", jo)), (("A", jo),))

        NP_ = RC // 2
        for it in range(NP_ + 2):
            if it < NP_:
                stA((2 * it, 2 * it + 1))
            if 1 <= it <= NP_:
                stB((2 * it - 2, 2 * it - 1))
            if 2 <= it <= NP_ + 1:
                stC((2 * it - 4, 2 * it - 3))
        yk = tuple(("A", jr) for jr in range(RC))
        for s_ in range(2):
            wb = self.ws_get("r_out")
            W = self.wview(wb, RC, 512)
            for mm_ in range(4):
                m = s_ * 4 + mm_
                b = self.bank()
                self.mm_group(self.PSb(b), [(W[:, jr, mm_ * 128:(mm_ + 1) * 128], self.AR[:, jr, :]) for jr in range(RC)],
                              yk + (("W", wb),), (("P", b),))
                self.tt("dve", self.X[:, m, :], self.X[:, m, :], self.PSb(b), ALU.add, (("P", b), ("X", m)), (("X", m),))

    def mlstm(self, L):
        P = self.P
        j = L // 2
        self.norm(PRM_OFF["mix_norm"] + L * 8)
        P.fence()
        MA, AR, PRM, PS = self.MA, self.AR, self.PRM, self.PS
        GI = MA[0:8, 0:512]
        NLF = MA[0:8, 512:1024]
        NBC = MA[0:8, 1024:1536]
        EF = MA[0:8, 1536:2048]
        FF = MA[0:8, 2048:2560]
        TMP8 = MA[0:8, 2560:3072]
        A8 = MA[0:8, 3072:3080]
        M8 = MA[0:8, 3080:3088]
        W8 = MA[0:8, 3088:3096]
        NEGBF = MA[0:8, 3096:3097]
        WD = MA[0:8, 3136:3200]
        ETW = MA[0:64, 3200:3392]
        ETOK = MA[:, 3200:3328].rearrange("p (c n) -> p c n", c=8)
        WBC = MA[:, 3328:3392].rearrange("p (c h) -> p c h", c=8)
        STMPS = [MA[:, 3392:3904], MA[:, 3904:4416]]
        DENS = [MA[:, 4416:4480], MA[:, 4480:4544]]
        TMPC = MA[0:64, 4544:5600].rearrange("p (h n) -> p h n", h=8)
        NUMS = [MA[:, 5632:6688], MA[:, 6688:7744]]
        SQ = MA[:, 7744:8768]
        xn_keys = tuple(("XN", c) for c in range(NCH))
        k = lambda n, *a: ("MA:" + n,) + a
        marr = self.MST[0:8, j, :]
        mk = ("MST", j)
        wb = self.ws_get("m_gate")
        WG = self.wview(wb, 8, 16)
        bi_, bf_ = self.bank(), self.bank()
        self.mm_group(self.PSb(bi_, rows=slice(0, 8)), [(WG[:, c, 0:8], self.XN[:, c, :]) for c in range(NCH)],
                      xn_keys + (("W", wb),), (("P", bi_),))
        self.mm_group(self.PSb(bf_, rows=slice(0, 8)), [(WG[:, c, 8:16], self.XN[:, c, :]) for c in range(NCH)],
                      xn_keys + (("W", wb),), (("P", bf_),))
        obi = PRM_OFF["m_b_i"] + j
        obf = PRM_OFF["m_b_f"] + j
        self.ts("dve", NEGBF, PRM[0:8, obf:obf + 1], -1.0, None, ALU.mult, None, ("PRM",), (k("NEGBF"),))
        self.act(NLF, self.PSb(bf_, rows=slice(0, 8)), AF.Exp, (("P", bf_), k("NEGBF")), (k("NLF"),), bias=NEGBF, scale=-1.0)
        self.act(NLF, NLF, AF.Ln, (k("NLF"), "CST"), (k("NLF"),), bias=self.c_one[0:8])
        P.op("dve", lambda e: e.tensor_tensor_scan(NBC, self.RMASK, NLF, 0.0, ALU.mult, ALU.add),
             (k("NLF"), "CST"), (k("NBC"),))
        self.stt("dve", GI, self.PSb(bi_, rows=slice(0, 8)), PRM[0:8, obi:obi + 1], NBC, ALU.add, ALU.add,
                 (("P", bi_), "PRM", k("NBC")), (k("GI"),))
        GI3 = GI.rearrange("p (c t) -> p c t", c=8)
        NBC3 = NBC.rearrange("p (c t) -> p c t", c=8)
        P.op("dve", lambda e: e.tensor_reduce(A8, GI3, AX.X, ALU.max), (k("GI"),), (k("A8"),))
        for c in range(8):
            self.tt("dve", M8[:, c:c + 1], marr[:, c:c + 1], A8[:, c:c + 1], ALU.max, (mk, k("A8")), (k("M8"),))
            self.tt("dve", marr[:, c + 1:c + 2], M8[:, c:c + 1], NBC3[:, c, 63:64], ALU.subtract, (k("M8"), k("NBC")), (mk,))
        self.tt("dve", W8, marr[:, 0:8], M8, ALU.subtract, (mk, k("M8")), (k("W8"),))
        self.act(W8, W8, AF.Exp, (k("W8"),), (k("W8"),))
        self.cp("dve", marr[:, 0:1], marr[:, 8:9], (mk, k("W8")), (mk,))
        M8b = bcast(M8, 2, 64)
        self.tt("dve", TMP8.rearrange("p (c t) -> p c t", c=8), GI3, M8b, ALU.subtract, (k("GI"), k("M8")), (k("TMP8"),))
        self.act(EF, TMP8, AF.Exp, (k("TMP8"),), (k("EF"),))
        self.tt("dve", FF.rearrange("p (c t) -> p c t", c=8), NBC3, M8b, ALU.subtract, (k("NBC"), k("M8")), (k("FF"),))
        self.act(FF, FF, AF.Exp, (k("FF"),), (k("FF"),))
        self.tt("dve", WD.rearrange("p (c h) -> p c h", c=8), bcast(W8, 2, 8), bcast(self.CST[0:8, C_ID:C_ID + 8], 1, 8), ALU.mult,
                (k("W8"), "CST"), (k("WD"),))
        MAB = MA[:, 8768:15400].bitcast(BF16)
        QK = MAB[:, 0:8192].rearrange("p (b n) -> p b n", b=16)
        HNBS = [MAB[:, 8192:9216], MAB[:, 9216:10240]]
        CBFS = [MAB[:, 10240 + i * 8 * VW: 10240 + (i + 1) * 8 * VW].rearrange("p (h n) -> p h n", h=8) for i in range(2)]
        R64 = slice(0, 64)
        wb = self.ws_get("m_in")
        W = self.wview(wb, 8, 512)
        for h in range(8):
            b = self.bank()
            self.mm_group(self.PSb(b, rows=R64), [(W[:, c, h * 64:(h + 1) * 64], self.XN[:, c, :]) for c in range(NCH)],
                          xn_keys + (("W", wb),), (("P", b),))
            P.op("act", lambda e, o=QK[R64, h, :], i=self.PSb(b, rows=R64): e.mul(o, i, 0.125), (("P", b),), (k("QK", h),))
        for vs in range(2):
            wb = self.ws_get("m_in")
            W = self.wview(wb, 8, 512)
            for c in range(8):
                b = self.bank()
                self.mm_group(self.PSb(b, rows=R64), [(self.XN[:, c_, c * 64:(c + 1) * 64], W[:, c_, :]) for c_ in range(NCH)],
                              xn_keys + (("W", wb),), (("P", b),))
                self.cp("act", self.VEXT[R64, c, vs * 4:(vs + 1) * 4, 0:128], self.PSb(b, rows=R64).rearrange("p (h v) -> p h v", h=4),
                        (("P", b),), (("VEXT", c),))
        for os_ in range(2):
            wb = self.ws_get("m_in")
            W = self.wview(wb, 8, 512)
            for jo in range(4):
                h = os_ * 4 + jo
                b = self.bank()
                self.mm_group(self.PSb(b), [(W[:, c, jo * 128:(jo + 1) * 128], self.XN[:, c, :]) for c in range(NCH)],
                              xn_keys + (("W", wb),), (("P", b),))
                self.act(AR[:, 8 + h, :], self.PSb(b), AF.Sigmoid, (("P", b),), (("A", 8 + h),))
                P.op("act", lambda e, o=AR[:, 8 + h, :], w=PRM[:, PRM_OFF["m_head_norm"] + j * 8 + h: PRM_OFF["m_head_norm"] + j * 8 + h + 1]: e.mul(o, o, w),
                     (("A", 8 + h), "PRM"), (("A", 8 + h),))
        bt_ = self.bank()
        I8 = self.CST[0:8, C_ID:C_ID + 8]
        for c in range(8):
            self.mm(self.PSb(bt_, rows=slice(0, 64), cols=slice(c * 16, c * 16 + 8)), EF[:, c * 64:(c + 1) * 64], I8,
                    (k("EF"), "CST"), (("P", bt_),))
            self.mm(self.PSb(bt_, rows=slice(0, 64), cols=slice(c * 16 + 8, c * 16 + 16)), FF[:, c * 64:(c + 1) * 64], I8,
                    (k("FF"), "CST"), (("P", bt_),))
        self.mm(self.PSb(bt_, rows=slice(0, 64), cols=slice(128, 192)), self.CST[0:8, C_ONES:C_ONES + 64], WD, (k("WD"), "CST"), (("P", bt_),))
        self.cp("dve", ETW, self.PSb(bt_, rows=slice(0, 64), cols=slice(0, 192)), (("P", bt_),), (k("ETW"),))
        wb = self.ws_get("m_in")
        W = self.wview(wb, 8, 512)
        for h in range(8):
            b = self.bank()
            self.mm_group(self.PSb(b, rows=R64), [(W[:, c, h * 64:(h + 1) * 64], self.XN[:, c, :]) for c in range(NCH)],
                          xn_keys + (("W", wb),), (("P", b),))
            self.cp("act", QK[R64, 8 + h, :], self.PSb(b, rows=R64), (("P", b),), (k("QK", 8 + h),))
        for c in range(8):
            b = self.bank()
            self.mm_group(self.PSb(b, rows=R64), [(self.XN[:, c_, c * 64:(c + 1) * 64], W[:, c_, :]) for c_ in range(NCH)],
                          xn_keys + (("W", wb),), (("P", b),))
            self.tt("dve", self.KTOK[R64, c, :].rearrange("p (h d) -> p h d", h=8),
                    self.PSb(b, rows=R64).rearrange("p (h d) -> p h d", h=8), bcast(ETOK[R64, c, 0:8], 2, 64), ALU.mult,
                    (("P", b), k("ETW")), (("KTOK", c),))
        CS = self.CSTATE
        csk = ("CSTATE", j)
        qk = tuple(k("QK", i) for i in range(16))
        pk = (("P", 4), ("P", 5), ("P", 6))
        dk = (("P", 1), ("P", 2), ("P", 3))
        def PD(h):
            o = 512 * (1 + h // 3) + (h % 3) * VW
            return PS[R64, o:o + 129]
        def PN(h):
            o = 512 * (4 + h // 3) + (h % 3) * VW
            return PS[R64, o:o + 129]
        def bankview(base, bnk):
            nh = 3 if bnk < 2 else 2
            return PS[R64, 512 * (base + bnk): 512 * (base + bnk) + nh * VW].rearrange("p (h n) -> p h n", h=nh), nh
        import os
        NCK = int(os.environ.get('DBG_NCHUNK', '8'))
        SIGH = AR[:, 8:16, :]
        YT = AR[:, 16:24, :]

        def S1a(c):
            tc0 = c * 64
            def fS(pe, tc0=tc0):
                ins = None
                for h in range(8):
                    ins = pe.matmul(PS[R64, h * 64:(h + 1) * 64], QK[R64, 8 + h, tc0:tc0 + 64], QK[R64, h, tc0:tc0 + 64], start=True, stop=True)
                return ins
            P.op("pe", fS, qk, (("P", 0),))

        def S1b(c):
            pc = c % 2
            self.tt("dve", STMPS[pc][R64, :].rearrange("p (h t) -> p h t", h=8), PS[R64, 0:512].rearrange("p (h t) -> p h t", h=8),
                    bcast(ETOK[R64, c, 0:8], 2, 64), ALU.mult, (("P", 0), k("ETW")), (k("STMP", pc),))
            self.tt("pool", self.SPT[R64, pc, :].rearrange("p (h t) -> p h t", h=8), STMPS[pc][R64, :].rearrange("p (h t) -> p h t", h=8),
                    bcast(self.MASK[R64, :], 1, 8), ALU.mult, (k("STMP", pc), "CST"), (("SPT", pc),))

        def S1(c):
            S1a(c)
            S1b(c)

        def evac(c):
            tc0 = c * 64
            self.tt("dve", YT[:, :, tc0:tc0 + 64], PS[:, 3584:4096].rearrange("p (h t) -> p h t", h=8), SIGH[:, :, tc0:tc0 + 64], ALU.mult,
                    (("P", 7),) + tuple(("A", 8 + h) for h in range(8)), tuple(("A", 16 + h) for h in range(8)))

        def fTop(c):
            HNB = HNBS[c % 2]
            def fT(pe, HNB=HNB):
                ins = None
                for h in range(8):
                    ins = pe.matmul(PS[:, 3584 + h * 64: 3584 + (h + 1) * 64], HNB[R64, h * 128:(h + 1) * 128], self.IDB[R64, 0:64], start=True, stop=True)
                return ins
            P.op("pe", fT, (k("HN", c % 2), "IDB"), (("P", 7),))

        S1(0)
        for c in range(NCK):
            pc, tc0 = c % 2, c * 64
            CBF, NUM, DEN, HNB = CBFS[pc], NUMS[pc], DENS[pc], HNBS[pc]
            NUM3 = NUM[R64, :].rearrange("p (h n) -> p h n", h=8)
            SQ3 = SQ[R64, :].rearrange("p (h v) -> p h v", h=8)
            d0, d1 = DEN[R64, 0:8], DEN[R64, 8:16]
            self.tt("dve", TMPC, CS[R64, j, :, :], bcast(WBC[R64, c, :], 2, VW), ALU.mult, (csk, k("ETW")), (k("TMPC"),))
            self.cp("act", CBF[R64, :, :], TMPC, (k("TMPC"),), (k("CBF", pc),))
            def fD(pe, c=c):
                ins = None
                for h in range(8):
                    ins = pe.matmul(PD(h), self.KTOK[R64, c, h * 64:(h + 1) * 64], self.VEXT[R64, c, h, 0:129], start=True, stop=True)
                return ins
            P.op("pe", fD, (("KTOK", c), ("VEXT", c)), dk)
            if c + 1 < NCK:
                S1a(c + 1)
            for bnk in range(3):
                v, nh = bankview(1, bnk)
                self.tt("dve", CS[R64, j, 3 * bnk:3 * bnk + nh, 0:129], TMPC[:, 3 * bnk:3 * bnk + nh, 0:129], v[:, :, 0:129], ALU.add,
                        (k("TMPC"), ("P", 1 + bnk)), (csk,))
            if c + 1 < NCK:
                S1b(c + 1)
            def fN(pe, c=c, tc0=tc0, pc=pc, CBF=CBF):
                ins = None
                for h in range(8):
                    pe.matmul(PN(h), self.SPT[R64, pc, h * 64:(h + 1) * 64], self.VEXT[R64, c, h, 0:129], start=True, stop=False)
                    ins = pe.matmul(PN(h), QK[R64, h, tc0:tc0 + 64], CBF[R64, h, 0:129], start=False, stop=True)
                return ins
            P.op("pe", fN, (("SPT", pc), ("VEXT", c), k("CBF", pc)) + qk, pk)
            if c > 0:
                fTop(c - 1)
            for bnk in range(3):
                v, nh = bankview(4, bnk)
                self.cp("act", NUM3[:, 3 * bnk:3 * bnk + nh, 0:129], v[:, :, 0:129], (("P", 4 + bnk),), (k("NUM", pc),))
            self.act(d0, NUM3[:, :, 128], AF.Abs, (k("NUM", pc),), (k("D0", pc),))
            if c > 0:
                evac(c - 1)
            self.tt("dve", d0, d0, ETOK[R64, c, 8:16], ALU.max, (k("D0", pc), k("ETW")), (k("D0", pc),))
            self.act(d0, d0, AF.Square, (k("D0", pc),), (k("D0", pc),), scale=float(np.sqrt(EPS)))
            self.tt("pool", SQ3, NUM3[:, :, 0:128], NUM3[:, :, 0:128], ALU.mult, (k("NUM", pc),), (k("SQ"),))
            P.op("dve", lambda e, d1=d1, SQ3=SQ3: e.tensor_reduce(d1, SQ3, AX.X, ALU.add), (k("SQ"),), (k("D1", pc),))
            self.stt("dve", d1, d1, 1.0 / 128.0, d0, ALU.mult, ALU.add, (k("D1", pc), k("D0", pc)), (k("D1", pc),))
            self.act(d1, d1, AF.Sqrt, (k("D1", pc),), (k("D1", pc),))
            P.op("dve", lambda e, d1=d1: e.reciprocal(d1, d1), (k("D1", pc),), (k("D1", pc),))
            self.tt("pool", HNB[R64, :].rearrange("p (h v) -> p h v", h=8), NUM3[:, :, 0:128], bcast(d1, 2, 128), ALU.mult,
                    (k("NUM", pc), k("D1", pc)), (k("HN", pc),))
        if NCK > 0:
            fTop(NCK - 1)
            evac(NCK - 1)
        yk = tuple(("A", 16 + h) for h in range(8))
        for s_ in range(2 if int(os.environ.get('DBG_NCHUNK', '8')) == 8 else 0):
            wb = self.ws_get("m_out")
            W = self.wview(wb, 8, 512)
            for mm_ in range(4):
                m = s_ * 4 + mm_
                b = self.bank()
                self.mm_group(self.PSb(b), [(W[:, h, mm_ * 128:(mm_ + 1) * 128], AR[:, 16 + h, :]) for h in range(8)],
                              yk + (("W", wb),), (("P", b),))
                self.tt("dve", self.X[:, m, :], self.X[:, m, :], self.PSb(b), ALU.add, (("P", b), ("X", m)), (("X", m),))


_CACHE = {}


def _get_nc(key, **kw):
    if key not in _CACHE:
        b = Builder(**kw)
        _CACHE[key] = (b, b.build())
    return _CACHE[key]


def kernel(**inputs):
    inp = {k: np.asarray(v) for k, v in inputs.items()}
    x = inp["x"]
    B = x.shape[0]
    b, nc = _get_nc("full")
    ws = pack_wstream(inp)
    prm = pack_params(inp)
    cst = pack_consts()
    in_maps = [{"x": np.ascontiguousarray(x[i]), "wstream": ws, "prm": prm, "cst": cst} for i in range(B)]
    res = run_bass_kernel_spmd(nc, in_maps, core_ids=list(range(B)))
    return np.stack([np.asarray(r["y"]) for r in res.results], axis=0).astype(np.float32)
```

```python
import numpy as np
from contextlib import ExitStack
import concourse.bass as bass
import concourse.mybir as mybir
from concourse.bass_utils import run_bass_kernel_spmd

F32 = mybir.dt.float32
BF16 = mybir.dt.bfloat16
AF = mybir.ActivationFunctionType
ALU = mybir.AluOpType
AX = mybir.AxisListType

D = 1024
DFF = 2816
SEQ = 4096
T = 512
NCH = 8
NJ = 22
HEADS = 8
RW = 1280
RC = 10
EPS = 1e-6
NB = 4
SLABW = 5632
VW = 132
NDMASEM = 24
SAME_ENG_SYNC = True

PRM_OFF = {}
def _prm_layout():
    off = 0
    for name, n in (("ff1_norm", 32), ("mix_norm", 32), ("ff2_norm", 32), ("final_norm", 8),
                    ("m_head_norm", 16), ("m_b_i", 2), ("m_b_f", 2), ("r_conv_w", 80),
                    ("r_conv_b", 20), ("r_b_a", 20), ("r_b_i", 20), ("r_lam", 20)):
        PRM_OFF[name] = off
        off += n
    return off
NPRM = _prm_layout()

C_ID, C_ONES, C_MASK, C_RMASK, C_MISC = 0, 128, 256, 320, 832
NCST = 848


def pack_consts():
    c = np.zeros((128, NCST), np.float32)
    c[:, C_ID:C_ID + 128] = np.eye(128, dtype=np.float32)
    c[:, C_ONES:C_ONES + 128] = 1.0
    s = np.arange(128)[:, None] % 64
    t = np.arange(64)[None, :]
    c[:, C_MASK:C_MASK + 64] = (s <= t).astype(np.float32)
    rm = np.ones(512, np.float32)
    rm[::64] = 0.0
    c[:, C_RMASK:C_RMASK + 512] = rm[None, :]
    c[:, C_MISC + 0] = EPS
    c[:, C_MISC + 1] = 1.0
    c[:, C_MISC + 2] = 0.0
    c[:, C_MISC + 3] = 0.25
    return c


def fm(v, nchunk):
    v = np.asarray(v, np.float32)
    lead = v.shape[:-1]
    v = v.reshape(lead + (nchunk, 128))
    v = np.moveaxis(v, -1, 0)
    return np.ascontiguousarray(v).reshape(128, -1)


def pack_params(inp):
    p = np.zeros((128, NPRM), np.float32)
    def put(name, arr):
        o = PRM_OFF[name]
        p[:arr.shape[0], o:o + arr.shape[1]] = arr
    put("ff1_norm", fm(inp["ff1_norm"], 8))
    put("mix_norm", fm(inp["mix_norm"], 8))
    put("ff2_norm", fm(inp["ff2_norm"], 8))
    put("final_norm", fm(inp["final_norm"], 8))
    put("m_head_norm", fm(inp["m_head_norm"], 8))
    put("m_b_i", np.asarray(inp["m_b_i"], np.float32).T.copy())
    put("m_b_f", np.asarray(inp["m_b_f"], np.float32).T.copy())
    put("r_conv_w", fm(np.asarray(inp["r_conv_w"], np.float32)[:, :, 0, :], 10))
    put("r_conv_b", fm(inp["r_conv_b"], 10))
    put("r_b_a", fm(inp["r_b_a"], 10))
    put("r_b_i", fm(inp["r_b_i"], 10))
    put("r_lam", fm(inp["r_lam"], 10))
    return p


def slab_plan():
    plan = []
    for L in range(4):
        for which in (1,):
            for s in range(11):
                plan.append(("ffin", L, (1, s), 8 * 512))
            for mp in range(4):
                plan.append(("ffout", L, (1, mp), 22 * 256))
        if L % 2 == 0:
            plan.append(("m_gate", L, 0, 8 * 16))
            for s in (0, 2, 3, 4, 5, 1):
                plan.append(("m_in", L, s, 8 * 512))
            for s in range(2):
                plan.append(("m_out", L, s, 8 * 512))
        else:
            for s in range(5):
                plan.append(("r_in", L, s, 8 * 512))
            for g in range(2):
                plan.append(("r_gate", L, g, 10 * 3 * 128))
            for s in range(2):
                plan.append(("r_out", L, s, 10 * 512))
        for s in range(11):
            plan.append(("ffin", L, (2, s), 8 * 512))
        for mp in range(4):
            plan.append(("ffout", L, (2, mp), 22 * 256))
    return plan


def _kslab(w, cols):
    sub = np.asarray(w)[:, cols]
    C = sub.shape[0] // 128
    return np.ascontiguousarray(sub.reshape(C, 128, sub.shape[1]).transpose(1, 0, 2)).reshape(128, -1)


def pack_wstream(inp):
    plan = slab_plan()
    tot = sum(p[3] for p in plan)
    ws = np.zeros((128, tot), np.float32)
    off = 0
    for kind, L, idx, size in plan:
        j = L // 2
        if kind == "ffin":
            which, s = idx
            w = inp["ff1_w_in" if which == 1 else "ff2_w_in"][L]
            cols = np.r_[s * 256:(s + 1) * 256, DFF + s * 256:DFF + (s + 1) * 256]
            blk = _kslab(w, cols)
        elif kind == "ffout":
            which, mp = idx
            w = inp["ff1_w_out" if which == 1 else "ff2_w_out"][L]
            blk = _kslab(w, np.r_[mp * 256:(mp + 1) * 256])
        elif kind == "m_gate":
            blk = _kslab(inp["m_w_in"][j], np.r_[3072:3088])
        elif kind == "m_in":
            blk = _kslab(inp["m_w_in"][j], np.r_[idx * 512:(idx + 1) * 512])
        elif kind == "m_out":
            blk = _kslab(inp["m_w_out"][j], np.r_[idx * 512:(idx + 1) * 512])
        elif kind == "r_in":
            blk = _kslab(inp["r_w_in"][j], np.r_[idx * 512:(idx + 1) * 512])
        elif kind == "r_out":
            blk = _kslab(inp["r_w_out"][j], np.r_[idx * 512:(idx + 1) * 512])
        elif kind == "r_gate":
            wg = np.asarray(inp["r_w_a" if idx == 0 else "r_w_i"][j], np.float32)
            dense = np.zeros((RW, RW), np.float32)
            for n in range(8):
                dense[n * 160:(n + 1) * 160, n * 160:(n + 1) * 160] = wg[n]
            blk = np.zeros((128, 10, 3, 128), np.float32)
            for jo in range(10):
                for di in range(3):
                    ji = jo + di - 1
                    if 0 <= ji < 10:
                        blk[:, jo, di, :] = dense[ji * 128:(ji + 1) * 128, jo * 128:(jo + 1) * 128]
            blk = blk.reshape(128, -1)
        assert blk.shape[1] == size, (kind, blk.shape, size)
        ws[:, off:off + size] = blk
        off += size
    return ws


class Op:
    __slots__ = ("eng", "fn", "waits", "sig", "ticket", "dma", "dsem", "dval", "idx")


class Prog:
    ENGS = ("pe", "act", "dve", "pool", "sp")

    def __init__(self):
        self.ops = []
        self.eng_ops = {e: [] for e in self.ENGS}
        self.last_w = {}
        self.readers = {}
        self.known = {e: {f: -1 for f in self.ENGS} for e in self.ENGS}
        self.known_dma = {e: set() for e in self.ENGS}
        self.vc = {}
        self.ndma = 0
        self.ndma_sw = 0
        self.dma_last = [None] * NDMASEM
        self.dma_cnt = [0] * NDMASEM
        self.fence_deps = set()
        self.fence_touched = set()

    def fence(self):
        col = set(self.fence_deps)
        for k in [k for k in self.last_w if isinstance(k, tuple) and str(k[0]).startswith("MA:")]:
            col.add(self.last_w.pop(k))
        for k in [k for k in self.readers if isinstance(k, tuple) and str(k[0]).startswith("MA:")]:
            col.update(self.readers.pop(k).values())
        best = {}
        keep = set()
        for d in col:
            o = self.ops[d]
            if o.dma:
                keep.add(d)
            else:
                best[o.eng] = max(best.get(o.eng, -1), d)
        keep.update(best.values())
        self.fence_deps = keep
        self.fence_touched = set()

    def op(self, eng, fn, reads=(), writes=(), dma=False):
        o = Op()
        o.eng, o.fn, o.dma, o.sig, o.ticket = eng, fn, dma, False, None
        o.idx = len(self.ops)
        deps = set()
        if self.fence_deps:
            for k in tuple(reads) + tuple(writes):
                if isinstance(k, tuple) and str(k[0]).startswith("MA:") and k not in self.fence_touched:
                    self.fence_touched.add(k)
                    deps.update(self.fence_deps)
        for k in reads:
            w = self.last_w.get(k)
            if w is not None:
                deps.add(w)
        for k in writes:
            w = self.last_w.get(k)
            if w is not None:
                deps.add(w)
            r = self.readers.get(k)
            if r:
                deps.update(r.values())
        if dma:
            if eng == "pool":
                slot = 8 + self.ndma_sw % (NDMASEM - 8)
                self.ndma_sw += 1
            else:
                slot = self.ndma % 8
                self.ndma += 1
            prev = self.dma_last[slot]
            if prev is not None:
                deps.add(prev)
            self.dma_last[slot] = o.idx
            self.dma_cnt[slot] += 16
            o.dsem, o.dval = slot, self.dma_cnt[slot]
        waits = []
        kn = self.known[eng]
        for d in sorted(deps):
            dop = self.ops[d]
            if dop.dma:
                if d in self.known_dma[eng]:
                    continue
                self.known_dma[eng].add(d)
                waits.append(d)
            else:
                f = dop.eng
                if f == eng:
                    if eng == "pe" or not SAME_ENG_SYNC:
                        continue
                if kn[f] >= d:
                    continue
                dop.sig = True
                waits.append(d)
                for g, v in self.vc[d].items():
                    if v > kn[g]:
                        kn[g] = v
        o.waits = waits
        if not dma:
            v = dict(kn)
            v[eng] = o.idx
            self.vc[o.idx] = v
        for k in reads:
            self.readers.setdefault(k, {})[("d", o.idx) if dma else eng] = o.idx
        for k in writes:
            self.last_w[k] = o.idx
            self.readers[k] = {}
        self.ops.append(o)
        self.eng_ops[eng].append(o)
        return o

    def emit(self, nc, es):
        sems = {e: es.enter_context(nc.semaphore("s_" + e)) for e in self.ENGS}
        dsems = [es.enter_context(nc.semaphore("d_%d" % i)) for i in range(NDMASEM)]
        for e in self.ENGS:
            t = 0
            for o in self.eng_ops[e]:
                if o.sig:
                    t += 1
                    o.ticket = t
            assert t < 60000, (e, t)
        for c in self.dma_cnt:
            assert c < 60000
        block = es.enter_context(nc.Block())
        ops = self.ops

        def run(eng_name):
            def body(eng):
                for o in self.eng_ops[eng_name]:
                    for d in o.waits:
                        dop = ops[d]
                        if dop.dma:
                            eng.wait_ge(dsems[dop.dsem], dop.dval)
                        else:
                            eng.wait_ge(sems[dop.eng], dop.ticket)
                    if o.fn is None:
                        continue
                    ins = o.fn(eng)
                    if o.dma:
                        ins.then_inc(dsems[o.dsem], 16)
                    elif o.sig:
                        ins.then_inc(sems[eng_name], 1)
            return body
        block.tensor(run("pe"))
        block.scalar(run("act"))
        block.vector(run("dve"))
        block.gpsimd(run("pool"))
        block.sync(run("sp"))


def bcast(ap, pos, n):
    pairs = [list(p) for p in ap.ap]
    pairs.insert(pos, [0, n])
    return bass.AP(ap.tensor, ap.offset, pairs)


class Builder:
    def __init__(self, layers=(0, 1, 2, 3), ntiles=8, parts=("f1", "mix", "f2"), final_norm=True):
        self.layers, self.ntiles, self.parts, self.final_norm = tuple(layers), ntiles, parts, final_norm
        self.P = Prog()
        self.plan = [p for p in slab_plan() if p[1] in self.layers and self._part_on(p)]
        self.full_plan = slab_plan()
        offs, o = [], 0
        for p in self.full_plan:
            offs.append(o)
            o += p[3]
        self.wtot = o
        self.plan_off = [offs[i] for i, p in enumerate(self.full_plan) if p[1] in self.layers and self._part_on(p)]
        self.ws_next = 0
        self.ws_issued = 0
        self.ws_total = len(self.plan) * ntiles
        self.pb = 0

    def _part_on(self, p):
        kind = p[0]
        if kind in ("ffin", "ffout"):
            return ("f1" if p[2][0] == 1 else "f2") in self.parts
        return "mix" in self.parts

    def bank(self):
        b = self.pb
        self.pb = (b + 1) % 8
        return b

    def PSb(self, b, rows=slice(0, 128), cols=slice(0, 512)):
        return self.PS[rows, b * 512 + cols.start: b * 512 + cols.stop]

    def mm_group(self, out, pairs, reads, writes, eng="pe"):
        n = len(pairs)
        def fn(pe, out=out, pairs=pairs, n=n):
            ins = None
            for i, (l, r) in enumerate(pairs):
                ins = pe.matmul(out, l, r, start=(i == 0), stop=(i == n - 1))
            return ins
        return self.P.op("pe", fn, reads, writes)

    def mm_chain(self, out, pairs, reads_each, common, writes):
        n = len(pairs)
        for i, (l, r) in enumerate(pairs):
            self.P.op("pe", lambda pe, l=l, r=r, i=i: pe.matmul(out, l, r, start=(i == 0), stop=(i == n - 1)),
                      tuple(reads_each[i]) + tuple(common), writes)

    def mm(self, out, l, r, reads, writes, start=True, stop=True):
        return self.P.op("pe", lambda pe: pe.matmul(out, l, r, start=start, stop=stop), reads, writes)

    def act(self, out, in_, func, reads, writes, bias=None, scale=None, accum_out=None):
        kw = {}
        if bias is not None:
            kw["bias"] = bias
        if scale is not None:
            kw["scale"] = scale
        if accum_out is not None:
            kw["accum_out"] = accum_out
        return self.P.op("act", lambda e: e.activation(out=out, in_=in_, func=func, **kw), reads, writes)

    def tt(self, eng, out, in0, in1, op, reads, writes):
        return self.P.op(eng, lambda e: e.tensor_tensor(out, in0, in1, op), reads, writes)

    def ts(self, eng, out, in0, s1, s2, op0, op1, reads, writes):
        if op1 is None:
            return self.P.op(eng, lambda e: e.tensor_scalar(out, in0, s1, None, op0), reads, writes)
        return self.P.op(eng, lambda e: e.tensor_scalar(out, in0, s1, s2, op0, op1), reads, writes)

    def stt(self, eng, out, in0, s, in1, op0, op1, reads, writes):
        return self.P.op(eng, lambda e: e.scalar_tensor_tensor(out, in0, s, in1, op0, op1), reads, writes)

    def cp(self, eng, out, in_, reads, writes):
        if eng == "act":
            return self.P.op("act", lambda e: e.copy(out, in_), reads, writes)
        return self.P.op(eng, lambda e: e.tensor_copy(out, in_), reads, writes)

    def ws_issue(self, k):
        n = len(self.plan)
        pi = k % n
        size = self.plan[pi][3]
        off = self.plan_off[pi]
        b = k % NB
        dst = self.WB[b][:, 0:size]
        src = self.wstream[:, off:off + size]
        self.P.op("pool", lambda g: g.dma_start(out=dst, in_=src), reads=(), writes=(("W", b),), dma=True)

    def ws_get(self, kind):
        k = self.ws_next
        self.ws_next += 1
        while self.ws_issued < min(k + NB - 1, self.ws_total):
            self.ws_issue(self.ws_issued)
            self.ws_issued += 1
        p = self.plan[k % len(self.plan)]
        assert p[0] == kind, (p, kind)
        return k % NB

    def wview(self, b, C, ncols):
        return self.WB[b][:, 0:C * ncols].rearrange("p (c n) -> p c n", c=C)

    def build(self):
        nc = bass.Bass("TRN2", target_bir_lowering=False)
        self.nc = nc
        ntok = self.ntiles * T
        self.x_d = nc.dram_tensor("x", [ntok, D], F32, kind="ExternalInput").ap()
        self.wstream = nc.dram_tensor("wstream", [128, self.wtot], F32, kind="ExternalInput").ap()
        self.prm_d = nc.dram_tensor("prm", [128, NPRM], F32, kind="ExternalInput").ap()
        self.cst_d = nc.dram_tensor("cst", [128, NCST], F32, kind="ExternalInput").ap()
        self.y_d = nc.dram_tensor("y", [ntok, D], F32, kind="ExternalOutput").ap()
        with ExitStack() as es:
            def sb(name, shape, dt):
                return es.enter_context(nc.sbuf_tensor(name, shape, dt))
            self.X = sb("X", [128, NCH, T], F32)
            self.XN = sb("XN", [128, NCH, T], BF16)
            self.AR = sb("AR", [128, 24, T], BF16)
            self.WB = [sb("WB%d" % i, [128, SLABW], BF16) for i in range(NB)]
            self.MA = sb("MA", [128, 15400], F32)
            self.PRM = sb("PRM", [128, NPRM], F32)
            self.CST = sb("CST", [128, NCST], F32)
            self.IDB = sb("IDB", [128, 128], BF16)
            self.ONESB = sb("ONESB", [128, 128], BF16)
            self.RS = sb("RS", [128, T], F32)
            self.DUM = sb("DUM", [128, 2], F32)
            self.SG = sb("SG", [128, 2, T], F32)
            self.VEXT = sb("VEXT", [64, 8, HEADS, VW], BF16)
            self.KTOK = sb("KTOK", [64, 8, 512], BF16)
            self.SPT = sb("SPT", [64, 2, 512], BF16)
            self.CSTATE = sb("CSTATE", [64, 2, HEADS, VW], F32)
            self.MST = sb("MST", [8, 2, 12], F32)
            self.RTAIL = sb("RTAIL", [128, 2, RC, 4], F32)
            self.RH = sb("RH", [128, 2, RC], F32)
            self.RSC = sb("RSC", [128, 2, 2, RC], F32)
            self.RHB = sb("RHB", [128, 2, 2, RC], F32)
            self.PS = es.enter_context(nc.psum_tensor("PS", [128, 4096], F32))
            self.record()
            self.P.emit(nc, es)
        return nc

    def record(self):
        P = self.P
        CST, PRM = self.CST, self.PRM
        P.op("sp", lambda q: q.dma_start(out=CST[:], in_=self.cst_d[:, :]), (), ("CST",), dma=True)
        P.op("sp", lambda q: q.dma_start(out=PRM[:], in_=self.prm_d[:, :]), (), ("PRM",), dma=True)
        self.ID = CST[:, C_ID:C_ID + 128]
        self.ONES = CST[:, C_ONES:C_ONES + 128]
        self.MASK = CST[:, C_MASK:C_MASK + 64]
        self.RMASK = CST[0:8, C_RMASK:C_RMASK + 512]
        self.c_eps = CST[:, C_MISC:C_MISC + 1]
        self.c_one = CST[:, C_MISC + 1:C_MISC + 2]
        self.c_zero = CST[:, C_MISC + 2:C_MISC + 3]
        self.c_quarter = CST[:, C_MISC + 3:C_MISC + 4]
        self.cp("dve", self.IDB[:], self.ID, ("CST",), ("IDB",))
        self.cp("dve", self.ONESB[:], self.ONES, ("CST",), ("ONESB",))
        P.op("dve", lambda e: e.memset(self.CSTATE[:], 0.0), (), (("CSTATE", 0), ("CSTATE", 1)))
        P.op("dve", lambda e: e.memset(self.MST[:], 0.0), (), (("MST", 0), ("MST", 1)))
        P.op("dve", lambda e: e.memset(self.RTAIL[:], 0.0), (), (("RTAIL", 0), ("RTAIL", 1)))
        P.op("dve", lambda e: e.memset(self.RH[:], 0.0), (), tuple(("RH", a, b) for a in range(2) for b in range(RC)))
        P.op("dve", lambda e: e.memset(self.VEXT[:], 1.0), (), tuple(("VEXT", i) for i in range(8)))
        if "mix" in self.parts and any(L % 2 for L in self.layers):
            self.rglru_consts()
        for ti in range(self.ntiles):
            self.load_x(ti)
            for L in self.layers:
                if "f1" in self.parts:
                    self.ffn(L, 1)
                if "mix" in self.parts:
                    if L % 2 == 0:
                        self.mlstm(L)
                    else:
                        self.rglru(L)
                if "f2" in self.parts:
                    self.ffn(L, 2)
            self.store_x(ti)
        P.op("sp", None, tuple(("YOUT", i) for i in range(4)), ())

    def load_x(self, ti):
        P = self.P
        P.fence()
        XS = self.MA[:, 0:4096].rearrange("p (b n) -> p b n", b=4)
        for tb in range(4):
            src = self.x_d[ti * T + tb * 128: ti * T + (tb + 1) * 128, :]
            dst = XS[:, tb, :]
            P.op("sp", lambda q, dst=dst, src=src: q.dma_start(out=dst, in_=src), (), (("MA:IO", tb),), dma=True)
        for c in range(NCH):
            b = self.bank()
            for tb in range(4):
                self.mm(self.PSb(b, cols=slice(tb * 128, (tb + 1) * 128)), XS[:, tb, c * 128:(c + 1) * 128], self.ID,
                        (("MA:IO", tb), "CST"), (("P", b),))
            self.cp("act" if c % 2 else "dve", self.X[:, c, :], self.PSb(b), (("P", b),), (("X", c),))

    def store_x(self, ti):
        P = self.P
        P.fence()
        if self.final_norm:
            self.norm(PRM_OFF["final_norm"], out_f32=True)
            src_key = "XF"
        OS = self.MA[:, 0:4096].rearrange("p (b n) -> p b n", b=4)
        XF = self.MA[:, 4096:8192].rearrange("p (c n) -> p c n", c=8)
        for tb in range(4):
            for half in range(2):
                b = self.bank()
                for cc in range(4):
                    c = half * 4 + cc
                    src = XF[:, c, tb * 128:(tb + 1) * 128] if self.final_norm else self.X[:, c, tb * 128:(tb + 1) * 128]
                    rk = ("MA:IO", 4 + c // 2) if self.final_norm else ("X", c)
                    self.mm(self.PSb(b, cols=slice(cc * 128, (cc + 1) * 128)), src, self.ID, (rk, "CST"), (("P", b),))
                self.cp("act" if half else "dve", OS[:, tb, half * 512:(half + 1) * 512], self.PSb(b), (("P", b),), (("MA:IO", tb),))
            dst = self.y_d[ti * T + tb * 128: ti * T + (tb + 1) * 128, :]
            src = OS[:, tb, :]
            P.op("sp", lambda q, dst=dst, src=src: q.dma_start(out=dst, in_=src), (("MA:IO", tb),), (("YOUT", tb),), dma=True)

    def norm(self, goff, out_f32=False):
        b = self.bank()
        self.act(self.DUM[:, 0:1], self.c_one, AF.Sqrt, ("CST",), ("DUM",))
        for c in range(NCH):
            self.act(self.AR[:, c, :], self.X[:, c, :], AF.Square, (("X", c),), (("A", c),))
        self.mm_chain(self.PSb(b), [(self.ONESB[:], self.AR[:, c, :]) for c in range(NCH)],
                      [(("A", c),) for c in range(NCH)], ("ONESB",), (("P", b),))
        self.act(self.RS[:], self.PSb(b), AF.Sqrt, (("P", b), "CST"), ("RS",), bias=self.c_eps, scale=1.0 / D)
        self.P.op("dve", lambda e: e.reciprocal(self.RS[:], self.RS[:]), ("RS",), ("RS",))
        XF = self.MA[:, 4096:8192].rearrange("p (c n) -> p c n", c=8)
        for c in range(NCH):
            g = self.PRM[:, goff + c: goff + c + 1]
            if out_f32:
                self.stt("dve", XF[:, c, :], self.X[:, c, :], g, self.RS[:], ALU.mult, ALU.mult,
                         (("X", c), "RS", "PRM"), (("MA:IO", 4 + c // 2),))
            else:
                self.stt("dve", self.XN[:, c, :], self.X[:, c, :], g, self.RS[:], ALU.mult, ALU.mult,
                         (("X", c), "RS", "PRM"), (("XN", c),))

    def ffn(self, L, which):
        self.norm(PRM_OFF["ff1_norm" if which == 1 else "ff2_norm"] + L * 8)
        xn_keys = tuple(("XN", c) for c in range(NCH))
        for s in range(11):
            wb = self.ws_get("ffin")
            W = self.wview(wb, 8, 512)
            for jj in range(2):
                j = 2 * s + jj
                bg, bu = self.bank(), self.bank()
                if j == 0:
                    self.mm_chain(self.PSb(bg), [(W[:, c, jj * 128:(jj + 1) * 128], self.XN[:, c, :]) for c in range(NCH)],
                                  [(("XN", c),) for c in range(NCH)], (("W", wb),), (("P", bg),))
                else:
                    self.mm_group(self.PSb(bg), [(W[:, c, jj * 128:(jj + 1) * 128], self.XN[:, c, :]) for c in range(NCH)],
                                  xn_keys + (("W", wb),), (("P", bg),))
                self.mm_group(self.PSb(bu), [(W[:, c, 256 + jj * 128:256 + (jj + 1) * 128], self.XN[:, c, :]) for c in range(NCH)],
                              xn_keys + (("W", wb),), (("P", bu),))
                self.act(self.SG[:, j % 2, :], self.PSb(bg), AF.Silu, (("P", bg),), (("SG", j % 2),))
                self.tt("dve", self.AR[:, j, :], self.SG[:, j % 2, :], self.PSb(bu), ALU.mult,
                        (("SG", j % 2), ("P", bu)), (("A", j),))
        hm_keys = tuple(("A", j) for j in range(NJ))
        NH = 18
        for mp in range(4):
            wb = self.ws_get("ffout")
            W = self.wview(wb, 22, 256)
            if mp == 0:
                bs = [self.bank(), self.bank()]
                prs = [[(W[:, j, mm_ * 128:(mm_ + 1) * 128], self.AR[:, j, :]) for j in range(NJ)] for mm_ in range(2)]
                for mm_ in range(2):
                    def f1(pe, prs=prs[mm_], o=self.PSb(bs[mm_])):
                        ins = None
                        for i in range(NH):
                            ins = pe.matmul(o, prs[i][0], prs[i][1], start=(i == 0), stop=False)
                        return ins
                    self.P.op("pe", f1, tuple(("A", j) for j in range(NH)) + (("W", wb),), (("P", bs[mm_]),))
                for mm_ in range(2):
                    for i in range(NH, NJ):
                        self.P.op("pe", lambda pe, i=i, prs=prs[mm_], o=self.PSb(bs[mm_]): pe.matmul(o, prs[i][0], prs[i][1], start=False, stop=(i == NJ - 1)),
                                  (("A", i), ("W", wb)), (("P", bs[mm_]),))
                    self.stt("dve", self.X[:, mm_, :], self.PSb(bs[mm_]), 0.5, self.X[:, mm_, :], ALU.mult, ALU.add,
                             (("P", bs[mm_]), ("X", mm_)), (("X", mm_),))
                continue
            for mm_ in range(2):
                m = 2 * mp + mm_
                b = self.bank()
                self.mm_group(self.PSb(b), [(W[:, j, mm_ * 128:(mm_ + 1) * 128], self.AR[:, j, :]) for j in range(NJ)],
                              hm_keys + (("W", wb),), (("P", b),))
                self.stt("dve", self.X[:, m, :], self.PSb(b), 0.5, self.X[:, m, :], ALU.mult, ALU.add,
                         (("P", b), ("X", m)), (("X", m),))

    def rglru_consts(self):
        o = PRM_OFF["r_lam"]
        lam = self.PRM[:, o:o + 20]
        r0 = self.RSC[:, 0].rearrange("p l c -> p (l c)")
        self.act(r0, lam, AF.Exp, ("PRM",), ("RSC",), scale=-1.0)
        self.act(r0, r0, AF.Ln, ("RSC", "CST"), ("RSC",), bias=self.c_one)
        self.ts("dve", r0, r0, -4.0, None, ALU.mult, None, ("RSC",), ("RSC",))
        for g, nm in ((0, "r_b_a"), (1, "r_b_i")):
            self.ts("dve", self.RHB[:, g].rearrange("p l c -> p (l c)"), self.PRM[:, PRM_OFF[nm]:PRM_OFF[nm] + 20], 0.5, None, ALU.mult, None,
                    ("PRM",), ("RHB",))

    def rglru(self, L):
        P = self.P
        j = L // 2
        self.norm(PRM_OFF["mix_norm"] + L * 8)
        P.fence()
        MA = self.MA
        UB = MA[:, 0:5160].rearrange("p (c n) -> p c n", c=RC)
        XC = MA[:, 5160:10280].rearrange("p (c n) -> p c n", c=RC)
        def SC(st, i):
            o = 10280 + (st * 5 + i) * 512
            return MA[:, o:o + 512]
        xn_keys = tuple(("XN", c) for c in range(NCH))
        self.cp("dve", UB[:, :, 0:4], self.RTAIL[:, j], (("RTAIL", j),), (("MA:UBT",),))
        for s_ in range(5):
            wb = self.ws_get("r_in")
            W = self.wview(wb, 8, 512)
            for qq in range(4):
                q = s_ * 4 + qq
                b = self.bank()
                if q == 0:
                    self.mm_chain(self.PSb(b), [(W[:, c, qq * 128:(qq + 1) * 128], self.XN[:, c, :]) for c in range(NCH)],
                                  [(("XN", c),) for c in range(NCH)], (("W", wb),), (("P", b),))
                else:
                    self.mm_group(self.PSb(b), [(W[:, c, qq * 128:(qq + 1) * 128], self.XN[:, c, :]) for c in range(NCH)],
                                  xn_keys + (("W", wb),), (("P", b),))
                if q < RC:
                    self.cp("act", UB[:, q, 4:516], self.PSb(b), (("P", b),), (("MA:UB", q),))
                else:
                    self.act(self.AR[:, q - RC, :], self.PSb(b), AF.Gelu_apprx_tanh, (("P", b),), (("A", q - RC),))
        ocw = PRM_OFF["r_conv_w"] + j * 40
        ocb = PRM_OFF["r_conv_b"] + j * 10
        def cwap(kk, jc):
            return self.PRM[:, ocw + kk * 10 + jc: ocw + kk * 10 + jc + 1]
        for jp in range(0, RC, 2):
            for jc in (jp, jp + 1):
                self.act(XC[:, jc, :], UB[:, jc, 1:513], AF.Identity, (("MA:UB", jc), ("MA:UBT",), "PRM"), (("MA:XC", jc),),
                         bias=self.PRM[:, ocb + jc: ocb + jc + 1], scale=cwap(0, jc))
            for kk in range(1, 4):
                for jc in (jp, jp + 1):
                    self.stt("dve", XC[:, jc, :], UB[:, jc, 1 + kk:513 + kk], cwap(kk, jc), XC[:, jc, :], ALU.mult, ALU.add,
                             (("MA:UB", jc), ("MA:UBT",), ("MA:XC", jc), "PRM"), (("MA:XC", jc),))
            for jc in (jp, jp + 1):
                self.ts("pool", self.AR[:, 10 + jc, :], XC[:, jc, :], 1.0, 0.0, ALU.mult, ALU.add, (("MA:XC", jc),), (("A", 10 + jc),))
        self.cp("dve", self.RTAIL[:, j], UB[:, :, 512:516], tuple(("MA:UB", jc) for jc in range(RC)), (("RTAIL", j),))
        P.fence()
        wa = self.ws_get("r_gate")
        wi = self.ws_get("r_gate")
        WA = self.WB[wa][:, 0:3840].rearrange("p (j d n) -> p j d n", j=RC, d=3)
        WI = self.WB[wi][:, 0:3840].rearrange("p (j d n) -> p j d n", j=RC, d=3)
        def SCR(st, i):
            n = st * 3 + i
            o = 10280 + n * 512 if n < 10 else (n - 10) * 512
            return MA[:, o:o + 512]
        hsc = lambda jo: self.RSC[:, 0, j, jo:jo + 1]
        KA = lambda jo: ("MA:SC", jo % 6, 0)
        KB = lambda jo: ("MA:SC", jo % 6, 1)
        KC = lambda jo: ("MA:SC", jo % 6, 2)
        BA = lambda jo: SCR(jo % 6, 0)
        BB = lambda jo: SCR(jo % 6, 1)
        BC = lambda jo: SCR(jo % 6, 2)

        def stA(jos):
            banks = {}
            for jo in jos:
                dis = [di for di in range(3) if 0 <= jo + di - 1 < RC]
                rk = tuple(("A", 10 + jo + di - 1) for di in dis)
                ba, bi = self.bank(), self.bank()
                banks[jo] = (ba, bi)
                self.mm_group(self.PSb(ba), [(WA[:, jo, di, :], self.AR[:, 10 + jo + di - 1, :]) for di in dis],
                              rk + (("W", wa),), (("P", ba),))
                self.mm_group(self.PSb(bi), [(WI[:, jo, di, :], self.AR[:, 10 + jo + di - 1, :]) for di in dis],
                              rk + (("W", wi),), (("P", bi),))
            for jo in jos:
                ba, bi = banks[jo]
                self.act(BA(jo), self.PSb(ba), AF.Tanh, (("P", ba), "RHB"), (KA(jo),), bias=self.RHB[:, 0, j, jo:jo + 1], scale=0.5)
                self.act(BB(jo), self.PSb(bi), AF.Tanh, (("P", bi), "RHB"), (KB(jo),), bias=self.RHB[:, 1, j, jo:jo + 1], scale=0.5)
            for jo in jos:
                self.act(BA(jo), BA(jo), AF.Identity, (KA(jo), "RSC"), (KA(jo),), bias=hsc(jo), scale=hsc(jo))

        def stB(jos):
            for jo in jos:
                self.ts("pool", BC(jo), BA(jo), 1.0 / 6.0, 0.5, ALU.mult, ALU.add, (KA(jo),), (KC(jo),))
            for jo in jos:
                self.tt("pool", BC(jo), BC(jo), BA(jo), ALU.mult, (KC(jo), KA(jo)), (KC(jo),))
            for jo in jos:
                self.stt("dve", BC(jo), BC(jo), 1.0, BA(jo), ALU.add, ALU.mult, (KC(jo), KA(jo)), (KC(jo),))
            for jo in jos:
                self.ts("pool", BA(jo), BC(jo), 1.0, 1.0, ALU.mult, ALU.add, (KC(jo),), (KA(jo),))
            for jo in jos:
                self.stt("dve", BB(jo), BB(jo), 1.0, XC[:, jo, :], ALU.add, ALU.mult, (KB(jo), ("MA:XC", jo)), (KB(jo),))
            for jo in jos:
                self.act(BC(jo), BA(jo), AF.Square, (KA(jo),), (KC(jo),))

        def stC(jos):
            for jo in jos:
                self.act(BC(jo), BC(jo), AF.Sqrt, (KC(jo), "CST"), (KC(jo),), bias=self.c_quarter, scale=-0.25)
            for jo in jos:
                self.tt("dve", BB(jo), BB(jo), BC(jo), ALU.mult, (KB(jo), KC(jo)), (KB(jo),))
            for jo in jos:
                hinit = self.RH[:, j, jo:jo + 1]
                P.op("dve", lambda e, o=BC(jo), a_=BA(jo), b_=BB(jo), hinit=hinit: e.tensor_tensor_scan(o, a_, b_, hinit, ALU.mult, ALU.add),
                     (KA(jo), KB(jo), ("RH", j, jo)), (KC(jo),))
            for jo in jos:
                self.cp("dve", self.RH[:, j, jo:jo + 1], BC(jo)[:, 511:512], (KC(jo),), (("RH", j, jo),))
                self.tt("dve", self.AR[:, jo, :], BC(jo), self.AR[:, jo, :], ALU.mult, (KC(jo), ("A", jo)), (("A", jo),))

        NP_ = RC // 2
        for it in range(NP_ + 2):
            if it < NP_:
                stA((2 * it, 2 * it + 1))
            if 1 <= it <= NP_:
                stB((2 * it - 2, 2 * it - 1))
            if 2 <= it <= NP_ + 1:
                stC((2 * it - 4, 2 * it - 3))
        yk = tuple(("A", jr) for jr in range(RC))
        for s_ in range(2):
            wb = self.ws_get("r_out")
            W = self.wview(wb, RC, 512)
            for mm_ in range(4):
                m = s_ * 4 + mm_
                b = self.bank()
                self.mm_group(self.PSb(b), [(W[:, jr, mm_ * 128:(mm_ + 1) * 128], self.AR[:, jr, :]) for jr in range(RC)],
                              yk + (("W", wb),), (("P", b),))
                self.tt("dve", self.X[:, m, :], self.X[:, m, :], self.PSb(b), ALU.add, (("P", b), ("X", m)), (("X", m),))

    def mlstm(self, L):
        P = self.P
        j = L // 2
        self.norm(PRM_OFF["mix_norm"] + L * 8)
        P.fence()
        MA, AR, PRM, PS = self.MA, self.AR, self.PRM, self.PS
        GI = MA[0:8, 0:512]
        NLF = MA[0:8, 512:1024]
        NBC = MA[0:8, 1024:1536]
        EF = MA[0:8, 1536:2048]
        FF = MA[0:8, 2048:2560]
        TMP8 = MA[0:8, 2560:3072]
        A8 = MA[0:8, 3072:3080]
        M8 = MA[0:8, 3080:3088]
        W8 = MA[0:8, 3088:3096]
        NEGBF = MA[0:8, 3096:3097]
        WD = MA[0:8, 3136:3200]
        ETW = MA[0:64, 3200:3392]
        ETOK = MA[:, 3200:3328].rearrange("p (c n) -> p c n", c=8)
        WBC = MA[:, 3328:3392].rearrange("p (c h) -> p c h", c=8)
        STMPS = [MA[:, 3392:3904], MA[:, 3904:4416]]
        DENS = [MA[:, 4416:4480], MA[:, 4480:4544]]
        TMPC = MA[0:64, 4544:5600].rearrange("p (h n) -> p h n", h=8)
        NUMS = [MA[:, 5632:6688], MA[:, 6688:7744]]
        SQ = MA[:, 7744:8768]
        xn_keys = tuple(("XN", c) for c in range(NCH))
        k = lambda n, *a: ("MA:" + n,) + a
        marr = self.MST[0:8, j, :]
        mk = ("MST", j)
        wb = self.ws_get("m_gate")
        WG = self.wview(wb, 8, 16)
        bi_, bf_ = self.bank(), self.bank()
        self.mm_chain(self.PSb(bi_, rows=slice(0, 8)), [(WG[:, c, 0:8], self.XN[:, c, :]) for c in range(NCH)],
                      [(("XN", c),) for c in range(NCH)], (("W", wb),), (("P", bi_),))
        self.mm_group(self.PSb(bf_, rows=slice(0, 8)), [(WG[:, c, 8:16], self.XN[:, c, :]) for c in range(NCH)],
                      xn_keys + (("W", wb),), (("P", bf_),))
        obi = PRM_OFF["m_b_i"] + j
        obf = PRM_OFF["m_b_f"] + j
        self.ts("dve", NEGBF, PRM[0:8, obf:obf + 1], -1.0, None, ALU.mult, None, ("PRM",), (k("NEGBF"),))
        self.act(NLF, self.PSb(bf_, rows=slice(0, 8)), AF.Exp, (("P", bf_), k("NEGBF")), (k("NLF"),), bias=NEGBF, scale=-1.0)
        self.act(NLF, NLF, AF.Ln, (k("NLF"), "CST"), (k("NLF"),), bias=self.c_one[0:8])
        P.op("dve", lambda e: e.tensor_tensor_scan(NBC, self.RMASK, NLF, 0.0, ALU.mult, ALU.add),
             (k("NLF"), "CST"), (k("NBC"),))
        self.stt("dve", GI, self.PSb(bi_, rows=slice(0, 8)), PRM[0:8, obi:obi + 1], NBC, ALU.add, ALU.add,
                 (("P", bi_), "PRM", k("NBC")), (k("GI"),))
        GI3 = GI.rearrange("p (c t) -> p c t", c=8)
        NBC3 = NBC.rearrange("p (c t) -> p c t", c=8)
        P.op("dve", lambda e: e.tensor_reduce(A8, GI3, AX.X, ALU.max), (k("GI"),), (k("A8"),))
        for c in range(8):
            self.tt("dve", M8[:, c:c + 1], marr[:, c:c + 1], A8[:, c:c + 1], ALU.max, (mk, k("A8")), (k("M8"),))
            self.tt("dve", marr[:, c + 1:c + 2], M8[:, c:c + 1], NBC3[:, c, 63:64], ALU.subtract, (k("M8"), k("NBC")), (mk,))
        self.tt("dve", W8, marr[:, 0:8], M8, ALU.subtract, (mk, k("M8")), (k("W8"),))
        self.act(W8, W8, AF.Exp, (k("W8"),), (k("W8"),))
        self.cp("dve", marr[:, 0:1], marr[:, 8:9], (mk, k("W8")), (mk,))
        M8b = bcast(M8, 2, 64)
        self.tt("dve", TMP8.rearrange("p (c t) -> p c t", c=8), GI3, M8b, ALU.subtract, (k("GI"), k("M8")), (k("TMP8"),))
        self.act(EF, TMP8, AF.Exp, (k("TMP8"),), (k("EF"),))
        self.tt("dve", FF.rearrange("p (c t) -> p c t", c=8), NBC3, M8b, ALU.subtract, (k("NBC"), k("M8")), (k("FF"),))
        self.act(FF, FF, AF.Exp, (k("FF"),), (k("FF"),))
        self.tt("dve", WD.rearrange("p (c h) -> p c h", c=8), bcast(W8, 2, 8), bcast(self.CST[0:8, C_ID:C_ID + 8], 1, 8), ALU.mult,
                (k("W8"), "CST"), (k("WD"),))
        MAB = MA[:, 8768:15400].bitcast(BF16)
        QK = MAB[:, 0:8192].rearrange("p (b n) -> p b n", b=16)
        HNBS = [MAB[:, 8192:9216], MAB[:, 9216:10240]]
        CBFS = [MAB[:, 10240 + i * 8 * VW: 10240 + (i + 1) * 8 * VW].rearrange("p (h n) -> p h n", h=8) for i in range(2)]
        R64 = slice(0, 64)
        wb = self.ws_get("m_in")
        W = self.wview(wb, 8, 512)
        for h in range(8):
            b = self.bank()
            self.mm_group(self.PSb(b, rows=R64), [(W[:, c, h * 64:(h + 1) * 64], self.XN[:, c, :]) for c in range(NCH)],
                          xn_keys + (("W", wb),), (("P", b),))
            P.op("act", lambda e, o=QK[R64, h, :], i=self.PSb(b, rows=R64): e.mul(o, i, 0.125), (("P", b),), (k("QK", h),))
        for vs in range(2):
            wb = self.ws_get("m_in")
            W = self.wview(wb, 8, 512)
            for c in range(8):
                b = self.bank()
                self.mm_group(self.PSb(b, rows=R64), [(self.XN[:, c_, c * 64:(c + 1) * 64], W[:, c_, :]) for c_ in range(NCH)],
                              xn_keys + (("W", wb),), (("P", b),))
                self.cp("act", self.VEXT[R64, c, vs * 4:(vs + 1) * 4, 0:128], self.PSb(b, rows=R64).rearrange("p (h v) -> p h v", h=4),
                        (("P", b),), (("VEXT", c),))
        for os_ in range(2):
            wb = self.ws_get("m_in")
            W = self.wview(wb, 8, 512)
            for jo in range(4):
                h = os_ * 4 + jo
                b = self.bank()
                self.mm_group(self.PSb(b), [(W[:, c, jo * 128:(jo + 1) * 128], self.XN[:, c, :]) for c in range(NCH)],
                              xn_keys + (("W", wb),), (("P", b),))
                self.act(AR[:, 8 + h, :], self.PSb(b), AF.Sigmoid, (("P", b),), (("A", 8 + h),))
                P.op("act", lambda e, o=AR[:, 8 + h, :], w=PRM[:, PRM_OFF["m_head_norm"] + j * 8 + h: PRM_OFF["m_head_norm"] + j * 8 + h + 1]: e.mul(o, o, w),
                     (("A", 8 + h), "PRM"), (("A", 8 + h),))
        bt_ = self.bank()
        I8 = self.CST[0:8, C_ID:C_ID + 8]
        for c in range(8):
            self.mm(self.PSb(bt_, rows=slice(0, 64), cols=slice(c * 16, c * 16 + 8)), EF[:, c * 64:(c + 1) * 64], I8,
                    (k("EF"), "CST"), (("P", bt_),))
            self.mm(self.PSb(bt_, rows=slice(0, 64), cols=slice(c * 16 + 8, c * 16 + 16)), FF[:, c * 64:(c + 1) * 64], I8,
                    (k("FF"), "CST"), (("P", bt_),))
        self.mm(self.PSb(bt_, rows=slice(0, 64), cols=slice(128, 192)), self.CST[0:8, C_ONES:C_ONES + 64], WD, (k("WD"), "CST"), (("P", bt_),))
        self.cp("dve", ETW, self.PSb(bt_, rows=slice(0, 64), cols=slice(0, 192)), (("P", bt_),), (k("ETW"),))
        wb = self.ws_get("m_in")
        W = self.wview(wb, 8, 512)
        for h in range(8):
            b = self.bank()
            self.mm_group(self.PSb(b, rows=R64), [(W[:, c, h * 64:(h + 1) * 64], self.XN[:, c, :]) for c in range(NCH)],
                          xn_keys + (("W", wb),), (("P", b),))
            self.cp("act", QK[R64, 8 + h, :], self.PSb(b, rows=R64), (("P", b),), (k("QK", 8 + h),))
        for c in range(8):
            b = self.bank()
            self.mm_group(self.PSb(b, rows=R64), [(self.XN[:, c_, c * 64:(c + 1) * 64], W[:, c_, :]) for c_ in range(NCH)],
                          xn_keys + (("W", wb),), (("P", b),))
            self.tt("dve", self.KTOK[R64, c, :].rearrange("p (h d) -> p h d", h=8),
                    self.PSb(b, rows=R64).rearrange("p (h d) -> p h d", h=8), bcast(ETOK[R64, c, 0:8], 2, 64), ALU.mult,
                    (("P", b), k("ETW")), (("KTOK", c),))
        CS = self.CSTATE
        csk = ("CSTATE", j)
        qk = tuple(k("QK", i) for i in range(16))
        pk = (("P", 4), ("P", 5), ("P", 6))
        dk = (("P", 1), ("P", 2), ("P", 3))
        def PD(h):
            o = 512 * (1 + h // 3) + (h % 3) * VW
            return PS[R64, o:o + 129]
        def PN(h):
            o = 512 * (4 + h // 3) + (h % 3) * VW
            return PS[R64, o:o + 129]
        def bankview(base, bnk):
            nh = 3 if bnk < 2 else 2
            return PS[R64, 512 * (base + bnk): 512 * (base + bnk) + nh * VW].rearrange("p (h n) -> p h n", h=nh), nh
        import os
        NCK = int(os.environ.get('DBG_NCHUNK', '8'))
        SIGH = AR[:, 8:16, :]
        YT = AR[:, 16:24, :]

        def S1a(c):
            tc0 = c * 64
            def fS(pe, tc0=tc0):
                ins = None
                for h in range(8):
                    ins = pe.matmul(PS[R64, h * 64:(h + 1) * 64], QK[R64, 8 + h, tc0:tc0 + 64], QK[R64, h, tc0:tc0 + 64], start=True, stop=True)
                return ins
            P.op("pe", fS, qk, (("P", 0),))

        def S1b(c):
            pc = c % 2
            self.tt("dve", STMPS[pc][R64, :].rearrange("p (h t) -> p h t", h=8), PS[R64, 0:512].rearrange("p (h t) -> p h t", h=8),
                    bcast(ETOK[R64, c, 0:8], 2, 64), ALU.mult, (("P", 0), k("ETW")), (k("STMP", pc),))
            self.tt("pool", self.SPT[R64, pc, :].rearrange("p (h t) -> p h t", h=8), STMPS[pc][R64, :].rearrange("p (h t) -> p h t", h=8),
                    bcast(self.MASK[R64, :], 1, 8), ALU.mult, (k("STMP", pc), "CST"), (("SPT", pc),))

        def S1(c):
            S1a(c)
            S1b(c)

        def evac(c):
            tc0 = c * 64
            self.tt("dve", YT[:, :, tc0:tc0 + 64], PS[:, 3584:4096].rearrange("p (h t) -> p h t", h=8), SIGH[:, :, tc0:tc0 + 64], ALU.mult,
                    (("P", 7),) + tuple(("A", 8 + h) for h in range(8)), tuple(("A", 16 + h) for h in range(8)))

        def fTop(c):
            HNB = HNBS[c % 2]
            def fT(pe, HNB=HNB):
                ins = None
                for h in range(8):
                    ins = pe.matmul(PS[:, 3584 + h * 64: 3584 + (h + 1) * 64], HNB[R64, h * 128:(h + 1) * 128], self.IDB[R64, 0:64], start=True, stop=True)
                return ins
            P.op("pe", fT, (k("HN", c % 2), "IDB"), (("P", 7),))

        S1(0)
        for c in range(NCK):
            pc, tc0 = c % 2, c * 64
            CBF, NUM, DEN, HNB = CBFS[pc], NUMS[pc], DENS[pc], HNBS[pc]
            NUM3 = NUM[R64, :].rearrange("p (h n) -> p h n", h=8)
            SQ3 = SQ[R64, :].rearrange("p (h v) -> p h v", h=8)
            d0, d1 = DEN[R64, 0:8], DEN[R64, 8:16]
            self.tt("dve", TMPC, CS[R64, j, :, :], bcast(WBC[R64, c, :], 2, VW), ALU.mult, (csk, k("ETW")), (k("TMPC"),))
            self.cp("act", CBF[R64, :, :], TMPC, (k("TMPC"),), (k("CBF", pc),))
            def fD(pe, c=c):
                ins = None
                for h in range(8):
                    ins = pe.matmul(PD(h), self.KTOK[R64, c, h * 64:(h + 1) * 64], self.VEXT[R64, c, h, 0:129], start=True, stop=True)
                return ins
            P.op("pe", fD, (("KTOK", c), ("VEXT", c)), dk)
            if c + 1 < NCK:
                S1a(c + 1)
            for bnk in range(3):
                v, nh = bankview(1, bnk)
                self.tt("dve", CS[R64, j, 3 * bnk:3 * bnk + nh, 0:129], TMPC[:, 3 * bnk:3 * bnk + nh, 0:129], v[:, :, 0:129], ALU.add,
                        (k("TMPC"), ("P", 1 + bnk)), (csk,))
            if c + 1 < NCK:
                S1b(c + 1)
            def fN(pe, c=c, tc0=tc0, pc=pc, CBF=CBF):
                ins = None
                for h in range(8):
                    pe.matmul(PN(h), self.SPT[R64, pc, h * 64:(h + 1) * 64], self.VEXT[R64, c, h, 0:129], start=True, stop=False)
                    ins = pe.matmul(PN(h), QK[R64, h, tc0:tc0 + 64], CBF[R64, h, 0:129], start=False, stop=True)
                return ins
            P.op("pe", fN, (("SPT", pc), ("VEXT", c), k("CBF", pc)) + qk, pk)
            if c > 0:
                fTop(c - 1)
            for bnk in range(3):
                v, nh = bankview(4, bnk)
                self.cp("act", NUM3[:, 3 * bnk:3 * bnk + nh, 0:129], v[:, :, 0:129], (("P", 4 + bnk),), (k("NUM", pc),))
            self.act(d0, NUM3[:, :, 128], AF.Abs, (k("NUM", pc),), (k("D0", pc),))
            if c > 0:
                evac(c - 1)
            self.tt("dve", d0, d0, ETOK[R64, c, 8:16], ALU.max, (k("D0", pc), k("ETW")), (k("D0", pc),))
            self.act(d0, d0, AF.Square, (k("D0", pc),), (k("D0", pc),), scale=float(np.sqrt(EPS)))
            self.tt("pool", SQ3, NUM3[:, :, 0:128], NUM3[:, :, 0:128], ALU.mult, (k("NUM", pc),), (k("SQ"),))
            P.op("dve", lambda e, d1=d1, SQ3=SQ3: e.tensor_reduce(d1, SQ3, AX.X, ALU.add), (k("SQ"),), (k("D1", pc),))
            self.stt("dve", d1, d1, 1.0 / 128.0, d0, ALU.mult, ALU.add, (k("D1", pc), k("D0", pc)), (k("D1", pc),))
            self.act(d1, d1, AF.Sqrt, (k("D1", pc),), (k("D1", pc),))
            P.op("dve", lambda e, d1=d1: e.reciprocal(d1, d1), (k("D1", pc),), (k("D1", pc),))
            self.tt("pool", HNB[R64, :].rearrange("p (h v) -> p h v", h=8), NUM3[:, :, 0:128], bcast(d1, 2, 128), ALU.mult,
                    (k("NUM", pc), k("D1", pc)), (k("HN", pc),))
        if NCK > 0:
            fTop(NCK - 1)
            evac(NCK - 1)
        yk = tuple(("A", 16 + h) for h in range(8))
        for s_ in range(2 if int(os.environ.get('DBG_NCHUNK', '8')) == 8 else 0):
            wb = self.ws_get("m_out")
            W = self.wview(wb, 8, 512)
            for mm_ in range(4):
                m = s_ * 4 + mm_
                b = self.bank()
                self.mm_group(self.PSb(b), [(W[:, h, mm_ * 128:(mm_ + 1) * 128], AR[:, 16 + h, :]) for h in range(8)],
                              yk + (("W", wb),), (("P", b),))
                self.tt("dve", self.X[:, m, :], self.X[:, m, :], self.PSb(b), ALU.add, (("P", b), ("X", m)), (("X", m),))


_CACHE = {}


def _get_nc(key, **kw):
    if key not in _CACHE:
        b = Builder(**kw)
        _CACHE[key] = (b, b.build())
    return _CACHE[key]


def kernel(**inputs):
    inp = {k: np.asarray(v) for k, v in inputs.items()}
    x = inp["x"]
    B = x.shape[0]
    b, nc = _get_nc("full")
    ws = pack_wstream(inp)
    prm = pack_params(inp)
    cst = pack_consts()
    in_maps = [{"x": np.ascontiguousarray(x[i]), "wstream": ws, "prm": prm, "cst": cst} for i in range(B)]
    res = run_bass_kernel_spmd(nc, in_maps, core_ids=list(range(B)))
    return np.stack([np.asarray(r["y"]) for r in res.results], axis=0).astype(np.float32)
```
